# Optimizing a Trainium2 kernel written in Bass

```python
import math
import jax, jax.numpy as jnp
from jax import lax
import numpy as np

D_MODEL = 4096
BATCH = 2
SEQ = 4096
DEPTH = 2

GRID_W = 64
CTX_LEN = 256
N_MIXERS = 2
N_HEADS = 32
HEAD_DIM = D_MODEL // N_HEADS
NA_KH_MAX = 8
NA_KW = 16
POOL_WINDOWS = (2, 4, 8, 16)
N_POOL_GROUPS = len(POOL_WINDOWS)
POOL_DG = D_MODEL // N_POOL_GROUPS
N_EXPERTS = 16
EC_CAPACITY_FACTOR = 2
D_FF_EXPERT = (3 * D_MODEL) // 8
N_ADA = 6
N_ATTN_LAYERS = (DEPTH + N_MIXERS - 1) // N_MIXERS
N_POOL_LAYERS = DEPTH // N_MIXERS
NORM_EPS = 1e-6
NEG_INF = -1e30

kernel_name = "hybrid_natten_pool_ecmoe_dit"


def rmsnorm(x, g):
    xf = x.astype(jnp.float32)
    y = xf * lax.rsqrt(jnp.mean(xf * xf, axis=-1, keepdims=True) + NORM_EPS)
    return (y * g.astype(jnp.float32)).astype(x.dtype)


def modulate(h, shift, scale):
    return h * (1 + scale) + shift


def ada_mods(cond, w, b):
    m = jnp.einsum('...d,de->...e', jax.nn.silu(cond), w) + b
    return jnp.split(m, N_ADA, axis=-1)


def qkv_heads(h, w_qkv, q_gain, k_gain):
    B, N, _ = h.shape
    qkv = jnp.einsum('bnd,de->bne', h, w_qkv).reshape(B, N, 3, N_HEADS, HEAD_DIM)
    q = rmsnorm(qkv[:, :, 0], q_gain)
    k = rmsnorm(qkv[:, :, 1], k_gain)
    v = qkv[:, :, 2]
    return q, k, v


def neighbourhood_attention(h_lat, h_ctx, w_qkv, q_gain, k_gain, rpb, w_out, with_ctx_queries):
    B, N, D = h_lat.shape
    n_ctx = h_ctx.shape[1]
    rows = N // GRID_W
    kh = min(NA_KH_MAX, rows)
    kw = min(NA_KW, GRID_W)
    scale = 1.0 / math.sqrt(HEAD_DIM)
    q, k, v = qkv_heads(h_lat, w_qkv, q_gain, k_gain)
    qc, kc, vc = qkv_heads(h_ctx, w_qkv, q_gain, k_gain)
    q_grid = q.reshape(B, rows, GRID_W, N_HEADS, HEAD_DIM)
    k_grid = k.reshape(B, rows, GRID_W, N_HEADS, HEAD_DIM)
    v_grid = v.reshape(B, rows, GRID_W, N_HEADS, HEAD_DIM)

    cols = jnp.arange(GRID_W)
    col_start = jnp.clip(cols - kw // 2, 0, GRID_W - kw)
    col_valid = (cols[None, :] >= col_start[:, None]) & (cols[None, :] < col_start[:, None] + kw)
    dc_idx = jnp.clip(cols[None, :] - cols[:, None] + (NA_KW - 1), 0, 2 * NA_KW - 2)

    def row_block(r):
        q_r = lax.dynamic_index_in_dim(q_grid, r, axis=1, keepdims=False)
        r0 = jnp.clip(r - kh // 2, 0, rows - kh)
        k_blk = lax.dynamic_slice_in_dim(k_grid, r0, kh, axis=1)
        v_blk = lax.dynamic_slice_in_dim(v_grid, r0, kh, axis=1)
        dr_idx = r0 + jnp.arange(kh) - r + (NA_KH_MAX - 1)
        bias = rpb[:, dr_idx[None, :, None], dc_idx[:, None, :]]
        s_lat = jnp.einsum('bqhd,bjkhd->bhqjk', q_r, k_blk).astype(jnp.float32) * scale
        s_lat = s_lat + bias.astype(jnp.float32)[None]
        s_lat = jnp.where(col_valid[:, None, :], s_lat, NEG_INF)
        s_ctx = jnp.einsum('bqhd,bchd->bhqc', q_r, kc).astype(jnp.float32) * scale
        s = jnp.concatenate([s_ctx, s_lat.reshape(B, N_HEADS, GRID_W, kh * GRID_W)], axis=-1)
        p = jax.nn.softmax(s, axis=-1).astype(v.dtype)
        o = jnp.einsum('bhqc,bchd->bqhd', p[..., :n_ctx], vc)
        o = o + jnp.einsum('bhqm,bmhd->bqhd', p[..., n_ctx:],
                           v_blk.reshape(B, kh * GRID_W, N_HEADS, HEAD_DIM))
        return o

    o = lax.map(row_block, jnp.arange(rows))
    o = jnp.moveaxis(o, 0, 1).reshape(B, N, D)
    out_lat = jnp.einsum('bnd,de->bne', o, w_out)
    out_ctx = None
    if with_ctx_queries:
        s = jnp.einsum('bqhd,bkhd->bhqk', qc, kc).astype(jnp.float32) * scale
        p = jax.nn.softmax(s, axis=-1).astype(vc.dtype)
        oc = jnp.einsum('bhqk,bkhd->bqhd', p, vc).reshape(B, n_ctx, D)
        out_ctx = jnp.einsum('bnd,de->bne', oc, w_out)
    return out_lat, out_ctx


def multiscale_pool(h, pool_w, pool_scale):
    B, N, D = h.shape
    hf = h.reshape(B, N, N_POOL_GROUPS, POOL_DG).astype(jnp.float32)
    csum = jnp.concatenate([jnp.zeros_like(hf[:, :1]), lax.cumsum(hf, axis=1)], axis=1)
    t = jnp.arange(N)[:, None]
    half = jnp.array(POOL_WINDOWS, jnp.int32)[None, :] // 2
    lo = jnp.clip(t - half, 0, N)
    hi = jnp.clip(t + half, 0, N)
    g_idx = jnp.arange(N_POOL_GROUPS)[None, :]
    win_sum = csum[:, hi, g_idx] - csum[:, lo, g_idx]
    count = (hi - lo).astype(jnp.float32)[None, :, :, None]
    pooled = (win_sum / count - hf).astype(h.dtype)
    y = jnp.einsum('bngc,gcd->bngd', pooled, pool_w).reshape(B, N, D)
    return y * pool_scale


def expert_choice_moe(h, w_router, w_gate, w_up, w_down):
    B, N, D = h.shape
    cap = max(1, EC_CAPACITY_FACTOR * N // N_EXPERTS)
    aff = jax.nn.softmax(jnp.einsum('bnd,de->bne', h, w_router).astype(jnp.float32), axis=-1)
    gates, idx = lax.top_k(jnp.swapaxes(aff, 1, 2), cap)
    b_idx = jnp.arange(B)[:, None, None]
    xs = h[b_idx, idx]
    hid = jax.nn.silu(jnp.einsum('becd,edf->becf', xs, w_gate)) * jnp.einsum('becd,edf->becf', xs, w_up)
    y = jnp.einsum('becf,efd->becd', hid, w_down) * gates[..., None].astype(h.dtype)
    return jnp.zeros_like(h).at[b_idx, idx].add(y)


def setup_inputs(seed: int = 0) -> dict:
    key = jax.random.key(seed)
    ks = jax.random.split(key, 20)

    def nrm(k, shape, s):
        return jax.random.normal(k, shape, jnp.float32) * s

    D = D_MODEL
    return {
        "x": nrm(ks[0], (BATCH, SEQ, D), 1.0),
        "c": nrm(ks[1], (BATCH, D), 1.0),
        "ctx": nrm(ks[2], (BATCH, CTX_LEN, D), 1.0),
        "c_ctx": nrm(ks[3], (D,), 1.0),
        "ada_w": nrm(ks[4], (DEPTH, D, N_ADA * D), 0.5 * D ** -0.5),
        "ada_b": nrm(ks[5], (DEPTH, N_ADA * D), 0.01),
        "norm_mix_g": 1.0 + nrm(ks[6], (DEPTH, D), 0.01),
        "norm_ffn_g": 1.0 + nrm(ks[7], (DEPTH, D), 0.01),
        "na_w_qkv": nrm(ks[8], (N_ATTN_LAYERS, D, 3 * D), D ** -0.5),
        "na_q_gain": 1.0 + nrm(ks[9], (N_ATTN_LAYERS, HEAD_DIM), 0.01),
        "na_k_gain": 1.0 + nrm(ks[10], (N_ATTN_LAYERS, HEAD_DIM), 0.01),
        "na_rpb": nrm(ks[11], (N_ATTN_LAYERS, N_HEADS, 2 * NA_KH_MAX - 1, 2 * NA_KW - 1), 0.1),
        "na_w_out": nrm(ks[12], (N_ATTN_LAYERS, D, D), D ** -0.5),
        "pool_w": nrm(ks[13], (N_POOL_LAYERS, N_POOL_GROUPS, POOL_DG, POOL_DG), POOL_DG ** -0.5),
        "pool_scale": 1.0 + nrm(ks[14], (N_POOL_LAYERS, D), 0.01),
        "moe_w_router": nrm(ks[15], (DEPTH, D, N_EXPERTS), D ** -0.5),
        "moe_w_gate": nrm(ks[16], (DEPTH, N_EXPERTS, D, D_FF_EXPERT), D ** -0.5),
        "moe_w_up": nrm(ks[17], (DEPTH, N_EXPERTS, D, D_FF_EXPERT), D ** -0.5),
        "moe_w_down": nrm(ks[18], (DEPTH, N_EXPERTS, D_FF_EXPERT, D), D_FF_EXPERT ** -0.5),
    }


def reference(x, c, ctx, c_ctx, ada_w, ada_b, norm_mix_g, norm_ffn_g, na_w_qkv, na_q_gain,
              na_k_gain, na_rpb, na_w_out, pool_w, pool_scale, moe_w_router, moe_w_gate,
              moe_w_up, moe_w_down):
    ctx_s = ctx
    for i in range(DEPTH):
        mixer = i % N_MIXERS
        slot = i // N_MIXERS
        ctx_needed_later = any(j % N_MIXERS == 0 for j in range(i + 1, DEPTH))
        sh_m, sc_m, g_m, sh_f, sc_f, g_f = ada_mods(c[:, None, :], ada_w[i], ada_b[i])
        h = modulate(rmsnorm(x, norm_mix_g[i]), sh_m, sc_m)
        use_ctx = (mixer == 0) or ctx_needed_later
        if use_ctx:
            csh_m, csc_m, cg_m, csh_f, csc_f, cg_f = ada_mods(c_ctx[None, None, :], ada_w[i], ada_b[i])
            hc = modulate(rmsnorm(ctx_s, norm_mix_g[i]), csh_m, csc_m)
        if mixer == 0:
            y, yc = neighbourhood_attention(h, hc, na_w_qkv[slot], na_q_gain[slot], na_k_gain[slot],
                                            na_rpb[slot], na_w_out[slot], ctx_needed_later)
        else:
            y = multiscale_pool(h, pool_w[slot], pool_scale[slot])
            yc = multiscale_pool(hc, pool_w[slot], pool_scale[slot]) if ctx_needed_later else None
        x = x + g_m * y
        hf = modulate(rmsnorm(x, norm_ffn_g[i]), sh_f, sc_f)
        x = x + g_f * expert_choice_moe(hf, moe_w_router[i], moe_w_gate[i], moe_w_up[i], moe_w_down[i])
        if ctx_needed_later:
            ctx_s = ctx_s + cg_m * yc
            hcf = modulate(rmsnorm(ctx_s, norm_ffn_g[i]), csh_f, csc_f)
            ctx_s = ctx_s + cg_f * expert_choice_moe(hcf, moe_w_router[i], moe_w_gate[i],
                                                     moe_w_up[i], moe_w_down[i])
    return x
```

```python
import numpy as np
import concourse.bass as bass
import concourse.mybir as mybir
from concourse.bass_utils import run_bass_kernel_spmd

F32 = mybir.dt.float32
BF16 = mybir.dt.bfloat16
AF = mybir.ActivationFunctionType
ALU = mybir.AluOpType
AX = mybir.AxisListType
NCORES = 8
ENGS = ("pe", "act", "dve", "pool", "sp")
NDS = 24


class Sch:
    def __init__(self, nc):
        self.nc = nc
        self.prog = {e: [] for e in ENGS}
        self.cnt = {e: 0 for e in ENGS}
        self.waited = {e: {} for e in ENGS}
        self.last_w = {}
        self.readers = {}
        self.dnext = 0
        self.dval = [0] * NDS
        self.out_events = []

    def _deps(self, eng, reads, writes):
        deps = []
        for r in reads:
            if r in self.last_w:
                deps.append(self.last_w[r])
        for w in writes:
            if w in self.last_w:
                deps.append(self.last_w[w])
            deps.extend(self.readers.get(w, []))
        need = {}
        for (s, v) in deps:
            if s == eng and eng == "pe":
                continue
            if v > need.get(s, 0):
                need[s] = v
        out = []
        for s, v in need.items():
            if self.waited[eng].get(s, 0) < v:
                self.waited[eng][s] = v
                out.append((s, v))
        return out

    def _commit(self, ev, reads, writes):
        for w in writes:
            self.last_w[w] = ev
            self.readers[w] = []
        for r in reads:
            if r not in writes:
                self.readers.setdefault(r, []).append(ev)

    def op(self, eng, fn, reads=(), writes=()):
        waits = self._deps(eng, reads, writes)
        self.cnt[eng] += 1
        ev = (eng, self.cnt[eng])
        self.prog[eng].append((waits, fn, eng, 1))
        self._commit(ev, reads, writes)
        return ev

    def dma(self, eng, out, in_, reads=(), writes=(), is_output=False):
        waits = self._deps(eng, reads, writes)
        j = self.dnext
        self.dnext = (self.dnext + 1) % NDS
        s = "d%d" % j
        if self.dval[j] > 0 and self.waited[eng].get(s, 0) < self.dval[j]:
            self.waited[eng][s] = self.dval[j]
            waits.append((s, self.dval[j]))
        self.dval[j] += 16
        ev = (s, self.dval[j])
        self.prog[eng].append((waits, lambda e, o=out, i=in_: e.dma_start(out=o, in_=i), s, 16))
        self._commit(ev, reads, writes)
        if is_output:
            self.out_events.append(ev)
        return ev

    def emit(self):
        nc = self.nc
        import contextlib
        with contextlib.ExitStack() as st:
            sems = {}
            for e in ENGS:
                sems[e] = st.enter_context(nc.semaphore("s_" + e))
            for j in range(NDS):
                sems["d%d" % j] = st.enter_context(nc.semaphore("s_d%d" % j))
            fin = {}
            for (s, v) in self.out_events:
                fin[s] = max(fin.get(s, 0), v)
            block = st.enter_context(nc.Block())

            def run(engobj, name):
                for (waits, fn, semname, inc) in self.prog[name]:
                    for (s, v) in waits:
                        engobj.wait_ge(sems[s], v)
                    fn(engobj).then_inc(sems[semname], inc)
                if name == "sp":
                    for s, v in fin.items():
                        engobj.wait_ge(sems[s], v)

            @block.tensor
            def _(e):
                run(e, "pe")

            @block.scalar
            def _(e):
                run(e, "act")

            @block.vector
            def _(e):
                run(e, "dve")

            @block.gpsimd
            def _(e):
                run(e, "pool")

            @block.sync
            def _(e):
                run(e, "sp")


_uid = [0]


def _sb(nc, st, shape, dt, name):
    _uid[0] += 1
    return st.enter_context(nc.sbuf_tensor("%s_%d" % (name, _uid[0]), shape, dt))


def _ps(nc, st, shape, dt, name):
    _uid[0] += 1
    return st.enter_context(nc.psum_tensor("%s_%d" % (name, _uid[0]), shape, dt))


def build_matmul(G, R, Kd, N, NT=512, fp32=False, swiglu=False, rowscale=False,
                 bias=False, silu_a=False):
    import contextlib
    nc = bass.Bass("TRN2", target_bir_lowering=False)
    KC = Kd // 128
    assert Kd % 128 == 0
    RT = (R + 127) // 128
    NTn = (N + NT - 1) // NT
    cdt = F32 if fp32 else BF16
    AT = nc.dram_tensor("AT", [G, Kd, R], F32, kind="ExternalInput").ap()
    W = nc.dram_tensor("W", [G, Kd, N], F32, kind="ExternalInput").ap()
    if swiglu:
        W2 = nc.dram_tensor("W2", [G, Kd, N], F32, kind="ExternalInput").ap()
    if rowscale:
        RS = nc.dram_tensor("RS", [G, 128, RT], F32, kind="ExternalInput").ap()
    if bias:
        BI = nc.dram_tensor("BI", [G, 1, N], F32, kind="ExternalInput").ap()
    Y = nc.dram_tensor("Y", [G, R, N], F32, kind="ExternalOutput").ap()
    S = Sch(nc)
    ldq = "sp" if fp32 else "pool"
    with contextlib.ExitStack() as st:
        at = _sb(nc, st, [128, KC, R], cdt, "at")
        nwb = 2
        wt = [_sb(nc, st, [128, KC, NT], cdt, "wt") for _ in range(nwb)]
        if swiglu:
            wt2 = [_sb(nc, st, [128, KC, NT], cdt, "wt2") for _ in range(nwb)]
            tmp = [_sb(nc, st, [128, NT], F32, "tmp") for _ in range(2)]
        if rowscale:
            rs = _sb(nc, st, [128, RT], F32, "rs")
        if bias:
            bi = _sb(nc, st, [1, N], F32, "bi")
            ones = _sb(nc, st, [1, 128], F32, "ones")
            S.op("dve", lambda e: e.memset(ones[:], 1.0), writes=["ones"])
        og = [_sb(nc, st, [128, NT], F32, "og") for _ in range(4)]
        npb = 2 if swiglu else 4
        pt = [_ps(nc, st, [128, NT], F32, "pt") for _ in range(npb)]
        if swiglu:
            pt2 = [_ps(nc, st, [128, NT], F32, "pt2") for _ in range(npb)]
        it = 0
        wi = 0
        ATK = ["at_%d" % k0 for k0 in range(0, KC, 8)]
        for g in range(G):
            for k0 in range(0, KC, 8):
                k1 = min(KC, k0 + 8)
                S.dma(ldq, at[:, k0:k1, :],
                      AT[g, k0 * 128:k1 * 128, :].rearrange("(kc p) r -> p kc r", p=128),
                      writes=["at_%d" % k0])
            if silu_a:
                S.op("act", lambda e: e.activation(out=at[:], in_=at[:], func=AF.Silu),
                     reads=ATK, writes=ATK)
            if rowscale:
                S.dma("sp", rs[:], RS[g], writes=["rs"])
            if bias:
                S.dma("sp", bi[:], BI[g], writes=["bi"])
            for nt in range(NTn):
                n0 = nt * NT
                nw = min(NT, N - n0)
                wb = wi % nwb
                wi += 1
                for k0 in range(0, KC, 8):
                    k1 = min(KC, k0 + 8)
                    S.dma(ldq, wt[wb][:, k0:k1, 0:nw],
                          W[g, k0 * 128:k1 * 128, n0:n0 + nw].rearrange("(kc p) n -> p kc n", p=128),
                          writes=["wt%d_%d" % (wb, k0)])
                    if swiglu:
                        S.dma(ldq, wt2[wb][:, k0:k1, 0:nw],
                              W2[g, k0 * 128:k1 * 128, n0:n0 + nw].rearrange("(kc p) n -> p kc n", p=128),
                              writes=["wt2%d_%d" % (wb, k0)])
                for rt in range(RT):
                    r0 = rt * 128
                    rw = min(128, R - r0)
                    pb = it % npb
                    ob = it % 4
                    it += 1

                    def mm(e, pt_=pt[pb], w_=wt[wb], r0=r0, rw=rw, nw=nw, n0=n0):
                        ins = None
                        for kc in range(KC):
                            ins = e.matmul(pt_[0:rw, 0:nw], at[:, kc, r0:r0 + rw], w_[:, kc, 0:nw],
                                           start=(kc == 0), stop=(kc == KC - 1 and not bias))
                        if bias:
                            ins = e.matmul(pt_[0:rw, 0:nw], ones[0:1, 0:rw], bi[0:1, n0:n0 + nw],
                                           start=False, stop=True)
                        return ins
                    rd = ATK + ["wt%d_%d" % (wb, k0) for k0 in range(0, KC, 8)] + (["ones", "bi"] if bias else [])
                    S.op("pe", mm, reads=rd, writes=["pt%d" % pb])
                    if swiglu:
                        def mm2(e, pt_=pt2[pb], w_=wt2[wb], r0=r0, rw=rw, nw=nw):
                            ins = None
                            for kc in range(KC):
                                ins = e.matmul(pt_[0:rw, 0:nw], at[:, kc, r0:r0 + rw], w_[:, kc, 0:nw],
                                               start=(kc == 0), stop=(kc == KC - 1))
                            return ins
                        S.op("pe", mm2, reads=ATK + ["wt2%d_%d" % (wb, k0) for k0 in range(0, KC, 8)], writes=["pt2%d" % pb])
                        tb = it % 2
                        S.op("act", lambda e, t_=tmp[tb], p_=pt[pb], rw=rw, nw=nw:
                             e.activation(out=t_[0:rw, 0:nw], in_=p_[0:rw, 0:nw], func=AF.Silu),
                             reads=["pt%d" % pb], writes=["tmp%d" % tb])
                        S.op("dve", lambda e, o_=og[ob], t_=tmp[tb], p_=pt2[pb], rw=rw, nw=nw:
                             e.tensor_tensor(out=o_[0:rw, 0:nw], in0=t_[0:rw, 0:nw], in1=p_[0:rw, 0:nw], op=ALU.mult),
                             reads=["tmp%d" % tb, "pt2%d" % pb], writes=["og%d" % ob])
                    elif rowscale:
                        S.op("dve", lambda e, o_=og[ob], p_=pt[pb], rw=rw, nw=nw, rt=rt:
                             e.tensor_scalar(out=o_[0:rw, 0:nw], in0=p_[0:rw, 0:nw], scalar1=rs[0:rw, rt:rt + 1],
                                             scalar2=None, op0=ALU.mult),
                             reads=["pt%d" % pb, "rs"], writes=["og%d" % ob])
                    else:
                        if it % 2 == 0:
                            S.op("act", lambda e, o_=og[ob], p_=pt[pb], rw=rw, nw=nw:
                                 e.activation(out=o_[0:rw, 0:nw], in_=p_[0:rw, 0:nw], func=AF.Copy),
                                 reads=["pt%d" % pb], writes=["og%d" % ob])
                        else:
                            S.op("dve", lambda e, o_=og[ob], p_=pt[pb], rw=rw, nw=nw:
                                 e.tensor_copy(out=o_[0:rw, 0:nw], in_=p_[0:rw, 0:nw]),
                                 reads=["pt%d" % pb], writes=["og%d" % ob])
                    S.dma("sp", Y[g, r0:r0 + rw, n0:n0 + nw], og[ob][0:rw, 0:nw],
                          reads=["og%d" % ob], is_output=True)
        S.emit()
    return nc


_prog_cache = {}


def run_prog(key, builder, in_maps):
    import time, sys
    t0 = time.time()
    if key not in _prog_cache:
        _prog_cache[key] = builder()
    nc = _prog_cache[key]
    t1 = time.time()
    res = run_bass_kernel_spmd(nc, in_maps, core_ids=list(range(NCORES)))
    nb = sum(v.nbytes for m in in_maps for v in m.values())
    print("[run_prog] %s build %.1fs run %.1fs in %.0f MB" % (str(key)[:60], t1 - t0, time.time() - t1, nb / 1e6),
          file=sys.stderr, flush=True)
    return res.results


def dev_matmul(ATs, Ws, W2s=None, RSs=None, BIs=None, fp32=False, NT=512, silu_a=False):
    G, Kd, R = ATs[0].shape
    N = Ws[0].shape[2]
    swiglu = W2s is not None
    rowscale = RSs is not None
    bias = BIs is not None
    key = ("mm", G, R, Kd, N, NT, fp32, swiglu, rowscale, bias, silu_a)
    maps = []
    for c in range(NCORES):
        m = {"AT": np.ascontiguousarray(ATs[c], np.float32), "W": np.ascontiguousarray(Ws[c], np.float32)}
        if swiglu:
            m["W2"] = np.ascontiguousarray(W2s[c], np.float32)
        if rowscale:
            m["RS"] = np.ascontiguousarray(RSs[c], np.float32)
        if bias:
            m["BI"] = np.ascontiguousarray(BIs[c], np.float32)
        maps.append(m)
    res = run_prog(key, lambda: build_matmul(G, R, Kd, N, NT=NT, fp32=fp32, swiglu=swiglu,
                                             rowscale=rowscale, bias=bias, silu_a=silu_a), maps)
    return [r["Y"] for r in res]


def build_normmod(R, D, GS, segs, residual, eps=1e-6):
    import contextlib
    nc = bass.Bass("TRN2", target_bir_lowering=False)
    RT = R // 128
    assert R % 128 == 0 and len(segs) == RT
    NSEG = max(segs) + 1
    NG = D // GS
    X = nc.dram_tensor("X", [R, D], F32, kind="ExternalInput").ap()
    VG = nc.dram_tensor("VG", [NSEG, D], F32, kind="ExternalInput").ap()
    VSC = nc.dram_tensor("VSC", [NSEG, D], F32, kind="ExternalInput").ap()
    VSH = nc.dram_tensor("VSH", [NSEG, D], F32, kind="ExternalInput").ap()
    if residual:
        YR = nc.dram_tensor("YR", [R, D], F32, kind="ExternalInput").ap()
        V1 = nc.dram_tensor("V1", [NSEG, D], F32, kind="ExternalInput").ap()
        V2 = nc.dram_tensor("V2", [NSEG, D], F32, kind="ExternalInput").ap()
        XN = nc.dram_tensor("XN", [R, D], F32, kind="ExternalOutput").ap()
    H = nc.dram_tensor("H", [R, D], F32, kind="ExternalOutput").ap()
    S = Sch(nc)
    with contextlib.ExitStack() as st:
        Am = [_sb(nc, st, [128, D], F32, "Am") for _ in range(NSEG)]
        Sh = [_sb(nc, st, [128, D], F32, "Sh") for _ in range(NSEG)]
        if residual:
            V12 = [_sb(nc, st, [128, D], F32, "V12") for _ in range(NSEG)]
        xt = [_sb(nc, st, [128, D], F32, "xt") for _ in range(2)]
        ht = [_sb(nc, st, [128, D], F32, "ht") for _ in range(2)]
        if residual:
            yt = [_sb(nc, st, [128, D], F32, "yt") for _ in range(2)]
        ss = [_sb(nc, st, [128, NG], F32, "ss") for _ in range(2)]
        rr = [_sb(nc, st, [128, NG], F32, "rr") for _ in range(2)]
        for s in range(NSEG):
            S.dma("sp", Am[s][:], VSC[s:s + 1, :].partition_broadcast(128), writes=["Am%d" % s])
            S.dma("sp", Sh[s][:], VG[s:s + 1, :].partition_broadcast(128), writes=["Sh%d" % s])
            S.op("dve", lambda e, a=Am[s], g=Sh[s]: e.scalar_tensor_tensor(
                out=a[:], in0=a[:], scalar=1.0, in1=g[:], op0=ALU.add, op1=ALU.mult),
                reads=["Am%d" % s, "Sh%d" % s], writes=["Am%d" % s])
            S.dma("sp", Sh[s][:], VSH[s:s + 1, :].partition_broadcast(128), reads=[], writes=["Sh%d" % s])
            if residual:
                S.dma("sp", V12[s][:], V1[s:s + 1, :].partition_broadcast(128), writes=["V12%d" % s])
                S.dma("sp", ht[0][:], V2[s:s + 1, :].partition_broadcast(128), writes=["ht0"])
                S.op("dve", lambda e, a=V12[s]: e.tensor_tensor(out=a[:], in0=a[:], in1=ht[0][:], op=ALU.mult),
                     reads=["V12%d" % s, "ht0"], writes=["V12%d" % s])
        for rt in range(RT):
            b = rt % 2
            sg = segs[rt]
            r0 = rt * 128
            xk, hk, yk, sk, rk = "xt%d" % b, "ht%d" % b, "yt%d" % b, "ss%d" % b, "rr%d" % b
            S.dma("sp", xt[b][:], X[r0:r0 + 128, :], writes=[xk])
            if residual:
                S.dma("sp", yt[b][:], YR[r0:r0 + 128, :], writes=[yk])
                S.op("pool", lambda e, b=b, sg=sg: e.tensor_tensor(out=yt[b][:], in0=yt[b][:], in1=V12[sg][:], op=ALU.mult),
                     reads=[yk, "V12%d" % sg], writes=[yk])
                S.op("pool", lambda e, b=b: e.tensor_tensor(out=xt[b][:], in0=xt[b][:], in1=yt[b][:], op=ALU.add),
                     reads=[xk, yk], writes=[xk])
                S.dma("sp", XN[r0:r0 + 128, :], xt[b][:], reads=[xk], is_output=True)
            S.op("dve", lambda e, b=b: e.tensor_tensor(out=ht[b][:], in0=xt[b][:], in1=xt[b][:], op=ALU.mult),
                 reads=[xk], writes=[hk])
            S.op("dve", lambda e, b=b: e.tensor_reduce(out=ss[b][:], in_=ht[b][:].rearrange("p (g d) -> p g d", d=GS),
                                                        axis=AX.X, op=ALU.add),
                 reads=[hk], writes=[sk])
            S.op("dve", lambda e, b=b: e.tensor_scalar(out=ss[b][:], in0=ss[b][:], scalar1=1.0 / GS, scalar2=eps,
                                                        op0=ALU.mult, op1=ALU.add),
                 reads=[sk], writes=[sk])
            S.op("act", lambda e, b=b: e.activation(out=rr[b][:], in_=ss[b][:], func=AF.Sqrt),
                 reads=[sk], writes=[rk])
            S.op("dve", lambda e, b=b: e.reciprocal(out=rr[b][:], in_=rr[b][:]), reads=[rk], writes=[rk])
            if NG == 1:
                S.op("dve", lambda e, b=b, sg=sg: e.scalar_tensor_tensor(
                    out=ht[b][:], in0=xt[b][:], scalar=rr[b][:, 0:1], in1=Am[sg][:], op0=ALU.mult, op1=ALU.mult),
                    reads=[xk, rk, "Am%d" % sg], writes=[hk])
            else:
                S.op("dve", lambda e, b=b: e.tensor_tensor(
                    out=ht[b][:].rearrange("p (g d) -> p g d", d=GS),
                    in0=xt[b][:].rearrange("p (g d) -> p g d", d=GS),
                    in1=rr[b][:].unsqueeze(2).to_broadcast([128, NG, GS]), op=ALU.mult),
                    reads=[xk, rk], writes=[hk])
                S.op("dve", lambda e, b=b, sg=sg: e.tensor_tensor(out=ht[b][:], in0=ht[b][:], in1=Am[sg][:], op=ALU.mult),
                     reads=[hk, "Am%d" % sg], writes=[hk])
            S.op("pool", lambda e, b=b, sg=sg: e.tensor_tensor(out=ht[b][:], in0=ht[b][:], in1=Sh[sg][:], op=ALU.add),
                 reads=[hk, "Sh%d" % sg], writes=[hk])
            S.dma("sp", H[r0:r0 + 128, :], ht[b][:], reads=[hk], is_output=True)
        S.emit()
    return nc


def dev_normmod(Xs, VG, VSC, VSH, segs, GS, YRs=None, V1=None, V2=None):
    R, D = Xs[0].shape
    residual = YRs is not None
    key = ("nm", R, D, GS, tuple(segs), residual)
    maps = []
    f = lambda a: np.ascontiguousarray(a, np.float32)
    for c in range(NCORES):
        m = {"X": f(Xs[c]), "VG": f(VG[c]), "VSC": f(VSC[c]), "VSH": f(VSH[c])}
        if residual:
            m.update({"YR": f(YRs[c]), "V1": f(V1[c]), "V2": f(V2[c])})
        maps.append(m)
    res = run_prog(key, lambda: build_normmod(R, D, GS, list(segs), residual), maps)
    return [r["H"] for r in res], ([r["XN"] for r in res] if residual else None)


NSLOT = 16
NPOOL = 37


def build_attn():
    import contextlib
    nc = bass.Bass("TRN2", target_bir_lowering=False)
    H, DH = 32, 128
    scale = 1.0 / np.sqrt(DH)
    NKT = NPOOL * 64 + 256
    QT = nc.dram_tensor("QT", [H, DH, NSLOT * 64], F32, kind="ExternalInput").ap()
    KT = nc.dram_tensor("KT", [H, DH, NKT], F32, kind="ExternalInput").ap()
    VA = nc.dram_tensor("VA", [H, 64, NPOOL, 129], F32, kind="ExternalInput").ap()
    VC = nc.dram_tensor("VC", [H, 128, 2, 129], F32, kind="ExternalInput").ap()
    BS = nc.dram_tensor("BS", [H, 64, 3, 512], F32, kind="ExternalInput").ap()
    O = nc.dram_tensor("O", [NSLOT, 64, H * DH], F32, kind="ExternalOutput").ap()
    S = Sch(nc)
    with contextlib.ExitStack() as st:
        qt = [_sb(nc, st, [128, NSLOT * 64], BF16, "qt") for _ in range(2)]
        kt = [_sb(nc, st, [128, NKT], BF16, "kt") for _ in range(2)]
        va = [_sb(nc, st, [64, NPOOL, 129], BF16, "va") for _ in range(2)]
        vc = [_sb(nc, st, [128, 2, 129], BF16, "vc") for _ in range(2)]
        bs = [_sb(nc, st, [64, 3, 512], F32, "bs") for _ in range(2)]
        ost = [_sb(nc, st, [64, NSLOT, DH], F32, "ost") for _ in range(2)]
        sb = [_sb(nc, st, [64, 512], F32, "sb") for _ in range(2)]
        pl = [_sb(nc, st, [64, 512], BF16, "pl") for _ in range(2)]
        pc = [_sb(nc, st, [128, 128], BF16, "pc") for _ in range(2)]
        rc = [_sb(nc, st, [64, 1], F32, "rc") for _ in range(2)]
        psl = [_ps(nc, st, [64, 512], F32, "psl") for _ in range(2)]
        psc = [_ps(nc, st, [128, 128], F32, "psc") for _ in range(2)]
        pso = [_ps(nc, st, [64, 129], F32, "pso") for _ in range(2)]
        it = 0
        for h in range(H):
            hb = h % 2
            S.dma("pool", qt[hb][:], QT[h], writes=["qt%d" % hb])
            S.dma("pool", kt[hb][:], KT[h], writes=["kt%d" % hb])
            S.dma("pool", va[hb][:], VA[h], writes=["va%d" % hb])
            S.dma("pool", vc[hb][:], VC[h], writes=["vc%d" % hb])
            S.dma("sp", bs[hb][:], BS[h], writes=["bs%d" % hb])
            for s in range(NSLOT):
                b = it % 2
                it += 1
                if s < 14:
                    prow0, typ = s, 0
                else:
                    prow0, typ = 21 + 8 * (s - 14), s - 13

                def mm_s(e, b=b, hb=hb, s=s, prow0=prow0):
                    ins = None
                    for j in range(8):
                        ins = e.matmul(psl[b][0:64, j * 64:(j + 1) * 64],
                                       kt[hb][:, (prow0 + j) * 64:(prow0 + j + 1) * 64],
                                       qt[hb][:, s * 64:(s + 1) * 64], start=True, stop=True)
                    return ins
                S.op("pe", mm_s, reads=["qt%d" % hb, "kt%d" % hb], writes=["psl%d" % b])

                def mm_c(e, b=b, hb=hb, s=s):
                    ins = None
                    for cb in range(2):
                        ins = e.matmul(psc[b][:, cb * 64:(cb + 1) * 64],
                                       kt[hb][:, NPOOL * 64 + cb * 128:NPOOL * 64 + (cb + 1) * 128],
                                       qt[hb][:, s * 64:(s + 1) * 64], start=True, stop=True)
                    return ins
                S.op("pe", mm_c, reads=["qt%d" % hb, "kt%d" % hb], writes=["psc%d" % b])
                S.op("dve", lambda e, b=b, hb=hb, typ=typ: e.scalar_tensor_tensor(
                    out=sb[b][:], in0=psl[b][:], scalar=float(scale), in1=bs[hb][:, typ, :],
                    op0=ALU.mult, op1=ALU.add),
                    reads=["psl%d" % b, "bs%d" % hb], writes=["sb%d" % b])
                S.op("act", lambda e, b=b: e.activation(out=pl[b][:], in_=sb[b][:], func=AF.Exp),
                     reads=["sb%d" % b], writes=["pl%d" % b])
                S.op("act", lambda e, b=b: e.activation(out=pc[b][:], in_=psc[b][:], func=AF.Exp, scale=float(scale)),
                     reads=["psc%d" % b], writes=["pc%d" % b])

                def mm_o(e, b=b, hb=hb, prow0=prow0):
                    ins = None
                    for j in range(8):
                        ins = e.matmul(pso[b][0:64, 0:129], pl[b][0:64, j * 64:(j + 1) * 64],
                                       va[hb][0:64, prow0 + j, :], start=(j == 0), stop=False)
                    for cb in range(2):
                        ins = e.matmul(pso[b][0:64, 0:129], pc[b][:, cb * 64:(cb + 1) * 64],
                                       vc[hb][:, cb, :], start=False, stop=(cb == 1))
                    return ins
                S.op("pe", mm_o, reads=["pl%d" % b, "pc%d" % b, "va%d" % hb, "vc%d" % hb], writes=["pso%d" % b])
                S.op("dve", lambda e, b=b: e.reciprocal(out=rc[b][:], in_=pso[b][0:64, 128:129]),
                     reads=["pso%d" % b], writes=["rc%d" % b])
                S.op("dve", lambda e, b=b, hb=hb, s=s: e.tensor_scalar(
                    out=ost[hb][:, s, :], in0=pso[b][0:64, 0:128], scalar1=rc[b][:, 0:1], scalar2=None, op0=ALU.mult),
                    reads=["pso%d" % b, "rc%d" % b], writes=["ost%d" % hb])
            S.dma("sp", O[:, :, h * DH:(h + 1) * DH].rearrange("s q d -> q s d"), ost[hb][:],
                  reads=["ost%d" % hb], is_output=True)
        S.emit()
    return nc


def dev_attn(maps):
    res = run_prog(("attn",), build_attn, maps)
    return [r["O"] for r in res]


def build_softmax(R, E):
    import contextlib
    nc = bass.Bass("TRN2", target_bir_lowering=False)
    T = R // 128
    X = nc.dram_tensor("X", [R, E], F32, kind="ExternalInput").ap()
    Y = nc.dram_tensor("Y", [R, E], F32, kind="ExternalOutput").ap()
    S = Sch(nc)
    with contextlib.ExitStack() as st:
        x = _sb(nc, st, [128, T, E], F32, "x")
        mx = _sb(nc, st, [128, T], F32, "mx")
        S.dma("sp", x[:], X.rearrange("(t p) e -> p t e", p=128), writes=["x"])
        S.op("dve", lambda e: e.tensor_reduce(out=mx[:], in_=x[:], axis=AX.X, op=ALU.max), reads=["x"], writes=["mx"])
        S.op("dve", lambda e: e.tensor_tensor(out=x[:], in0=x[:], in1=mx[:].unsqueeze(2).to_broadcast([128, T, E]),
                                              op=ALU.subtract), reads=["x", "mx"], writes=["x"])
        S.op("act", lambda e: e.activation(out=x[:], in_=x[:], func=AF.Exp), reads=["x"], writes=["x"])
        S.op("dve", lambda e: e.tensor_reduce(out=mx[:], in_=x[:], axis=AX.X, op=ALU.add), reads=["x"], writes=["mx"])
        S.op("dve", lambda e: e.reciprocal(out=mx[:], in_=mx[:]), reads=["mx"], writes=["mx"])
        S.op("dve", lambda e: e.tensor_tensor(out=x[:], in0=x[:], in1=mx[:].unsqueeze(2).to_broadcast([128, T, E]),
                                              op=ALU.mult), reads=["x", "mx"], writes=["x"])
        S.dma("sp", Y.rearrange("(t p) e -> p t e", p=128), x[:], reads=["x"], is_output=True)
        S.emit()
    return nc


def build_thresh(NR, NTOK, CAP, iters=36):
    import contextlib
    nc = bass.Bass("TRN2", target_bir_lowering=False)
    A = nc.dram_tensor("A", [NR, NTOK], F32, kind="ExternalInput").ap()
    M = nc.dram_tensor("M", [NR, NTOK], F32, kind="ExternalOutput").ap()
    S = Sch(nc)
    with contextlib.ExitStack() as st:
        a = _sb(nc, st, [NR, NTOK], F32, "a")
        c = _sb(nc, st, [NR, NTOK], F32, "c")
        lo = _sb(nc, st, [NR, 1], F32, "lo")
        hi = _sb(nc, st, [NR, 1], F32, "hi")
        mid = _sb(nc, st, [NR, 1], F32, "mid")
        cnt = _sb(nc, st, [NR, 1], F32, "cnt")
        d = _sb(nc, st, [NR, 1], F32, "d")
        S.dma("sp", a[:], A, writes=["a"])
        S.op("dve", lambda e: e.memset(lo[:], 0.0), writes=["lo"])
        S.op("dve", lambda e: e.memset(hi[:], 2.0), writes=["hi"])
        for _ in range(iters):
            S.op("dve", lambda e: e.tensor_tensor(out=mid[:], in0=lo[:], in1=hi[:], op=ALU.add),
                 reads=["lo", "hi"], writes=["mid"])
            S.op("dve", lambda e: e.tensor_scalar(out=mid[:], in0=mid[:], scalar1=0.5, scalar2=None, op0=ALU.mult),
                 reads=["mid"], writes=["mid"])
            S.op("dve", lambda e: e.tensor_scalar(out=c[:], in0=a[:], scalar1=mid[:, 0:1], scalar2=None, op0=ALU.is_ge),
                 reads=["a", "mid"], writes=["c"])
            S.op("dve", lambda e: e.tensor_reduce(out=cnt[:], in_=c[:], axis=AX.X, op=ALU.add),
                 reads=["c"], writes=["cnt"])
            S.op("dve", lambda e: e.tensor_scalar(out=cnt[:], in0=cnt[:], scalar1=float(CAP) - 0.5, scalar2=None,
                                                   op0=ALU.is_ge), reads=["cnt"], writes=["cnt"])
            S.op("dve", lambda e: e.tensor_tensor(out=d[:], in0=mid[:], in1=lo[:], op=ALU.subtract),
                 reads=["mid", "lo"], writes=["d"])
            S.op("dve", lambda e: e.scalar_tensor_tensor(out=lo[:], in0=d[:], scalar=cnt[:, 0:1], in1=lo[:],
                                                          op0=ALU.mult, op1=ALU.add),
                 reads=["d", "cnt", "lo"], writes=["lo"])
            S.op("dve", lambda e: e.tensor_tensor(out=d[:], in0=hi[:], in1=mid[:], op=ALU.subtract),
                 reads=["hi", "mid"], writes=["d"])
            S.op("dve", lambda e: e.scalar_tensor_tensor(out=hi[:], in0=d[:], scalar=cnt[:, 0:1], in1=mid[:],
                                                          op0=ALU.mult, op1=ALU.add),
                 reads=["d", "cnt", "mid"], writes=["hi"])
        S.op("dve", lambda e: e.tensor_scalar(out=c[:], in0=a[:], scalar1=lo[:, 0:1], scalar2=None, op0=ALU.is_ge),
             reads=["a", "lo"], writes=["c"])
        S.dma("sp", M, c[:], reads=["c"], is_output=True)
        S.emit()
    return nc


def dev_softmax(Xs):
    R, E = Xs[0].shape
    res = run_prog(("sm", R, E), lambda: build_softmax(R, E),
                   [{"X": np.ascontiguousarray(x, np.float32)} for x in Xs])
    return [r["Y"] for r in res]


def dev_thresh(As, cap):
    NR, NTOK = As[0].shape
    res = run_prog(("th", NR, NTOK, cap), lambda: build_thresh(NR, NTOK, cap),
                   [{"A": np.ascontiguousarray(a, np.float32)} for a in As])
    return [r["M"] for r in res]


D = 4096
NTOKB = 4096
GRID = 64
NH, DHD = 32, 128
NE, CAP, DFF = 16, 512, 1536
LMAX = 2560
POOLW = (2, 4, 8, 16)
ATT_INT0 = (4, 18, 32, 46)
ATT_SP = ((0, 1), (2, 3), (60, 61), (62, 63))
_EMU = [False]


def _T(a):
    return np.ascontiguousarray(np.swapaxes(a, -1, -2))


def _attn_rows(q):
    rows = [ATT_INT0[q] + s for s in range(14)] + list(ATT_SP[q])
    pool = [ATT_INT0[q] - 4 + w for w in range(21)]
    for r in ATT_SP[q]:
        r0 = min(max(r - 4, 0), GRID - 8)
        pool += [r0 + j for j in range(8)]
    return rows, pool


def _bias_tables(rpb, q):
    cols = np.arange(GRID)
    cstart = np.clip(cols - 8, 0, GRID - 16)
    valid = (cols[None, :] >= cstart[:, None]) & (cols[None, :] < cstart[:, None] + 16)
    dc = np.clip(cols[None, :] - cols[:, None] + 15, 0, 30)
    out = np.empty((NH, 64, 3, 512), np.float32)
    types = [None] + list(ATT_SP[q])
    for t, r in enumerate(types):
        if r is None:
            dr = np.arange(8) + 3
        else:
            r0 = min(max(r - 4, 0), GRID - 8)
            dr = r0 + np.arange(8) - r + 7
        B = rpb[:, dr[:, None, None], dc[None, :, :]]
        B = np.where(valid[None, None], B, np.float32(-30000.0))
        out[:, :, t, :] = B.transpose(0, 3, 1, 2).reshape(NH, 64, 512)
    return out


def _moe(hf, x_in, gvec, l, inp):
    c8 = range(NCORES)
    logits = dev_matmul([_T(hf[1024 * c:1024 * c + 1024])[None] for c in c8],
                        [inp["moe_w_router"][l][None]] * NCORES, fp32=True, NT=16)
    aff = np.concatenate(dev_softmax([y[0] for y in logits]), 0)
    AFT = aff.reshape(2, NTOKB, NE).transpose(0, 2, 1).reshape(2 * NE, NTOKB)
    mask = np.concatenate(dev_thresh([AFT[4 * c:4 * c + 4] for c in c8], CAP), 0)
    idx = np.zeros((2, NE, CAP), np.int64)
    for b in range(2):
        for e in range(NE):
            nz = np.nonzero(mask[b * NE + e] > 0.5)[0]
            idx[b, e, :min(CAP, len(nz))] = nz[:CAP]
    gidx = idx + (np.arange(2) * NTOKB)[:, None, None]
    ATs, RSs = [], []
    for c in c8:
        a = np.stack([np.concatenate([hf[gidx[0, e]], hf[gidx[1, e]]], 0) for e in (2 * c, 2 * c + 1)])
        ATs.append(_T(a))
        g = np.stack([np.concatenate([aff[gidx[0, e], e], aff[gidx[1, e], e]]) for e in (2 * c, 2 * c + 1)])
        RSs.append(g.reshape(2, 8, 128).transpose(0, 2, 1))
    hid = dev_matmul(ATs, [inp["moe_w_gate"][l][2 * c:2 * c + 2] for c in c8],
                     W2s=[inp["moe_w_up"][l][2 * c:2 * c + 2] for c in c8], NT=256)
    y = dev_matmul([_T(h) for h in hid], [inp["moe_w_down"][l][2 * c:2 * c + 2] for c in c8], RSs=RSs)
    yall = np.stack(y).reshape(NE, 2, CAP, D)
    ATc, Wc = [], []
    for c in c8:
        b, t0 = c // 4, (c % 4) * 1024
        ee, ss = np.nonzero((idx[b] >= t0) & (idx[b] < t0 + 1024))
        n = min(len(ee), LMAX)
        ee, ss = ee[:n], ss[:n]
        yl = np.zeros((LMAX, D), np.float32)
        yl[:n] = yall[ee, b, ss]
        sel = np.zeros((LMAX, 1024), np.float32)
        sel[np.arange(n), idx[b, ee, ss] - t0] = 1.0
        ATc.append(sel[None])
        Wc.append(yl[None])
    out = dev_matmul(ATc, Wc, fp32=True, NT=256)
    return np.concatenate([o[0] for o in out], 0)


def kernel(x, c, ctx, c_ctx, ada_w, ada_b, norm_mix_g, norm_ffn_g, na_w_qkv, na_q_gain, na_k_gain,
           na_rpb, na_w_out, pool_w, pool_scale, moe_w_router, moe_w_gate, moe_w_up, moe_w_down):
    inp = dict(moe_w_router=np.asarray(moe_w_router), moe_w_gate=np.asarray(moe_w_gate),
               moe_w_up=np.asarray(moe_w_up), moe_w_down=np.asarray(moe_w_down))
    f = lambda a: np.asarray(a, np.float32)
    x = f(x).reshape(2 * NTOKB, D)
    ctxf = f(ctx).reshape(512, D)
    c8 = range(NCORES)
    ones = np.ones(D, np.float32)
    zeros = np.zeros(D, np.float32)
    cond = np.stack([f(c)[0], f(c)[1], f(c_ctx)])
    ATa = np.stack([cond.T, cond.T])
    ada_w = np.asarray(ada_w)
    ada_b = f(ada_b)
    Ya = dev_matmul([ATa] * NCORES, [ada_w[:, :, 3072 * k:3072 * (k + 1)] for k in c8],
                    BIs=[ada_b[:, None, 3072 * k:3072 * (k + 1)] for k in c8], fp32=True, silu_a=True)
    mods = np.concatenate(Ya, -1).reshape(2, 3, 6, D)
    g_mix, g_ffn = f(norm_mix_g), f(norm_ffn_g)

    m0 = mods[0]
    Xs, VG, VSC, VSH = [], [], [], []
    for k in c8:
        b = k // 4
        Xs.append(np.concatenate([x[1024 * k:1024 * k + 1024], ctxf[64 * k:64 * k + 64], np.zeros((64, D), np.float32)]))
        VG.append(np.stack([g_mix[0], g_mix[0]]))
        VSC.append(np.stack([m0[b, 1], m0[2, 1]]))
        VSH.append(np.stack([m0[b, 0], m0[2, 0]]))
    Hh, _ = dev_normmod(Xs, VG, VSC, VSH, [0] * 8 + [1], D)
    Wq = np.asarray(na_w_qkv)[0][None]
    qkv = dev_matmul([_T(h)[None] for h in Hh], [Wq] * NCORES)
    qkv_lat = np.concatenate([y[0][:1024] for y in qkv], 0)
    qkv_ctx = np.concatenate([y[0][1024:1088] for y in qkv], 0)
    qg, kg = np.tile(f(na_q_gain)[0], NH), np.tile(f(na_k_gain)[0], NH)
    Xs = []
    for k in c8:
        sl = slice(1024 * k, 1024 * k + 1024)
        Xs.append(np.concatenate([qkv_lat[sl, 0:D], qkv_lat[sl, D:2 * D], qkv_ctx[64 * k:64 * k + 64, D:2 * D],
                                  np.zeros((64, D), np.float32)]))
    Hn, _ = dev_normmod(Xs, [np.stack([qg, kg])] * NCORES, [np.stack([zeros, zeros])] * NCORES,
                        [np.stack([zeros, zeros])] * NCORES, [0] * 8 + [1] * 9, DHD)
    qn = np.concatenate([h[0:1024] for h in Hn], 0)
    kn = np.concatenate([h[1024:2048] for h in Hn], 0)
    kcn = np.concatenate([h[2048:2112] for h in Hn], 0)
    vl = qkv_lat[:, 2 * D:3 * D]
    vcx = qkv_ctx[:, 2 * D:3 * D]
    rpb = f(na_rpb)[0]
    maps = []
    for k in c8:
        b, q = k // 4, k % 4
        rows, pool = _attn_rows(q)
        gq = qn[b * NTOKB:(b + 1) * NTOKB].reshape(GRID, GRID, NH, DHD)
        gk = kn[b * NTOKB:(b + 1) * NTOKB].reshape(GRID, GRID, NH, DHD)
        gv = vl[b * NTOKB:(b + 1) * NTOKB].reshape(GRID, GRID, NH, DHD)
        QT = gq[rows].transpose(2, 3, 0, 1).reshape(NH, DHD, NSLOT * 64)
        KTl = gk[pool].transpose(2, 3, 0, 1).reshape(NH, DHD, NPOOL * 64)
        KTc = kcn[256 * b:256 * b + 256].reshape(256, NH, DHD).transpose(1, 2, 0)
        VA = np.ones((NH, 64, NPOOL, 129), np.float32)
        VA[..., :128] = gv[pool].transpose(2, 1, 0, 3)
        VC = np.ones((NH, 128, 2, 129), np.float32)
        VC[..., :128] = vcx[256 * b:256 * b + 256].reshape(2, 128, NH, DHD).transpose(2, 1, 0, 3)
        maps.append({"QT": np.ascontiguousarray(QT), "KT": np.ascontiguousarray(np.concatenate([KTl, KTc], -1)),
                     "VA": VA, "VC": VC, "BS": _bias_tables(rpb, q)})
    Oc = dev_attn(maps)
    o_all = np.zeros((2, GRID, GRID, D), np.float32)
    for k in c8:
        rows, _ = _attn_rows(k % 4)
        o_all[k // 4, rows] = Oc[k]
    o_all = o_all.reshape(2 * NTOKB, D)
    Wo = np.asarray(na_w_out)[0][None]
    ya = dev_matmul([_T(o_all[1024 * k:1024 * k + 1024])[None] for k in c8], [Wo] * NCORES)
    hf, x1 = dev_normmod([x[1024 * k:1024 * k + 1024] for k in c8],
                         [g_ffn[0][None]] * NCORES, [m0[k // 4, 4][None] for k in c8], [m0[k // 4, 3][None] for k in c8],
                         [0] * 8, D, YRs=[y[0] for y in ya], V1=[m0[k // 4, 2][None] for k in c8], V2=[ones[None]] * NCORES)
    hf, x1 = np.concatenate(hf, 0), np.concatenate(x1, 0)
    mo = _moe(hf, x1, None, 0, inp)

    m1 = mods[1]
    h1, x2 = dev_normmod([x1[1024 * k:1024 * k + 1024] for k in c8],
                         [g_mix[1][None]] * NCORES, [m1[k // 4, 1][None] for k in c8], [m1[k // 4, 0][None] for k in c8],
                         [0] * 8, D, YRs=[mo[1024 * k:1024 * k + 1024] for k in c8],
                         V1=[m0[k // 4, 5][None] for k in c8], V2=[ones[None]] * NCORES)
    h1, x2 = np.concatenate(h1, 0), np.concatenate(x2, 0)
    ATp, Wp = [], []
    for k in c8:
        b, n0 = k // 4, (k % 4) * 1024
        pos = np.arange(n0 - 64, n0 + 1088)
        inb = (pos >= 0) & (pos < NTOKB)
        hh = np.zeros((1152, D), np.float32)
        hh[inb] = h1[b * NTOKB + pos[inb]]
        A = np.zeros((4, 1152, 1024), np.float32)
        n = n0 + np.arange(1024)
        for g, w in enumerate(POOLW):
            lo = np.clip(n - w // 2, 0, NTOKB)
            hi = np.clip(n + w // 2, 0, NTOKB)
            inv = (1.0 / (hi - lo)).astype(np.float32)
            for t in range(1024):
                A[g, lo[t] - (n0 - 64):hi[t] - (n0 - 64), t] = inv[t]
                A[g, n[t] - (n0 - 64), t] -= 1.0
        ATp.append(A)
        Wp.append(np.ascontiguousarray(hh.reshape(1152, 4, 1024).transpose(1, 0, 2)))
    pooled = dev_matmul(ATp, Wp, fp32=True)
    yp = dev_matmul([_T(p) for p in pooled], [np.asarray(pool_w)[0]] * NCORES)
    yp = [np.ascontiguousarray(y.transpose(1, 0, 2).reshape(1024, D)) for y in yp]
    ps = f(pool_scale)[0]
    hf, x3 = dev_normmod([x2[1024 * k:1024 * k + 1024] for k in c8],
                         [g_ffn[1][None]] * NCORES, [m1[k // 4, 4][None] for k in c8], [m1[k // 4, 3][None] for k in c8],
                         [0] * 8, D, YRs=yp, V1=[m1[k // 4, 2][None] for k in c8], V2=[ps[None]] * NCORES)
    hf, x3 = np.concatenate(hf, 0), np.concatenate(x3, 0)
    mo = _moe(hf, x3, None, 1, inp)
    _, x4 = dev_normmod([x3[1024 * k:1024 * k + 1024] for k in c8],
                        [ones[None]] * NCORES, [zeros[None]] * NCORES, [zeros[None]] * NCORES,
                        [0] * 8, D, YRs=[mo[1024 * k:1024 * k + 1024] for k in c8],
                        V1=[m1[k // 4, 5][None] for k in c8], V2=[ones[None]] * NCORES)
    return np.concatenate(x4, 0).reshape(2, NTOKB, D).astype(np.float32)
```

```python
import numpy as np
import concourse.bass as bass
import concourse.mybir as mybir
from concourse.bass_utils import run_bass_kernel_spmd

F32 = mybir.dt.float32
BF16 = mybir.dt.bfloat16
AF = mybir.ActivationFunctionType
ALU = mybir.AluOpType
AX = mybir.AxisListType
NCORES = 8
ENGS = ("pe", "act", "dve", "pool", "sp")
NDS = 24


class Sch:
    def __init__(self, nc):
        self.nc = nc
        self.prog = {e: [] for e in ENGS}
        self.cnt = {e: 0 for e in ENGS}
        self.waited = {e: {} for e in ENGS}
        self.last_w = {}
        self.readers = {}
        self.dnext = 0
        self.dval = [0] * NDS
        self.out_events = []

    def _deps(self, eng, reads, writes):
        deps = []
        for r in reads:
            if r in self.last_w:
                deps.append(self.last_w[r])
        for w in writes:
            if w in self.last_w:
                deps.append(self.last_w[w])
            deps.extend(self.readers.get(w, []))
        need = {}
        for (s, v) in deps:
            if s == eng and eng == "pe":
                continue
            if v > need.get(s, 0):
                need[s] = v
        out = []
        for s, v in need.items():
            if self.waited[eng].get(s, 0) < v:
                self.waited[eng][s] = v
                out.append((s, v))
        return out

    def _commit(self, ev, reads, writes):
        for w in writes:
            self.last_w[w] = ev
            self.readers[w] = []
        for r in reads:
            if r not in writes:
                self.readers.setdefault(r, []).append(ev)

    def op(self, eng, fn, reads=(), writes=()):
        waits = self._deps(eng, reads, writes)
        self.cnt[eng] += 1
        ev = (eng, self.cnt[eng])
        self.prog[eng].append((waits, fn, eng, 1))
        self._commit(ev, reads, writes)
        return ev

    def dma(self, eng, out, in_, reads=(), writes=(), is_output=False):
        waits = self._deps(eng, reads, writes)
        j = self.dnext
        self.dnext = (self.dnext + 1) % NDS
        s = "d%d" % j
        if self.dval[j] > 0 and self.waited[eng].get(s, 0) < self.dval[j]:
            self.waited[eng][s] = self.dval[j]
            waits.append((s, self.dval[j]))
        self.dval[j] += 16
        ev = (s, self.dval[j])
        self.prog[eng].append((waits, lambda e, o=out, i=in_: e.dma_start(out=o, in_=i), s, 16))
        self._commit(ev, reads, writes)
        if is_output:
            self.out_events.append(ev)
        return ev

    def emit(self):
        nc = self.nc
        import contextlib
        with contextlib.ExitStack() as st:
            sems = {}
            for e in ENGS:
                sems[e] = st.enter_context(nc.semaphore("s_" + e))
            for j in range(NDS):
                sems["d%d" % j] = st.enter_context(nc.semaphore("s_d%d" % j))
            fin = {}
            for (s, v) in self.out_events:
                fin[s] = max(fin.get(s, 0), v)
            block = st.enter_context(nc.Block())

            def run(engobj, name):
                for (waits, fn, semname, inc) in self.prog[name]:
                    for (s, v) in waits:
                        engobj.wait_ge(sems[s], v)
                    fn(engobj).then_inc(sems[semname], inc)
                if name == "sp":
                    for s, v in fin.items():
                        engobj.wait_ge(sems[s], v)

            @block.tensor
            def _(e):
                run(e, "pe")

            @block.scalar
            def _(e):
                run(e, "act")

            @block.vector
            def _(e):
                run(e, "dve")

            @block.gpsimd
            def _(e):
                run(e, "pool")

            @block.sync
            def _(e):
                run(e, "sp")


_uid = [0]


def _sb(nc, st, shape, dt, name):
    _uid[0] += 1
    return st.enter_context(nc.sbuf_tensor("%s_%d" % (name, _uid[0]), shape, dt))


def _ps(nc, st, shape, dt, name):
    _uid[0] += 1
    return st.enter_context(nc.psum_tensor("%s_%d" % (name, _uid[0]), shape, dt))


def build_matmul(G, R, Kd, N, NT=512, fp32=False, swiglu=False, rowscale=False,
                 bias=False, silu_a=False):
    import contextlib
    nc = bass.Bass("TRN2", target_bir_lowering=False)
    KC = Kd // 128
    assert Kd % 128 == 0
    RT = (R + 127) // 128
    NTn = (N + NT - 1) // NT
    cdt = F32 if fp32 else BF16
    AT = nc.dram_tensor("AT", [G, Kd, R], F32, kind="ExternalInput").ap()
    W = nc.dram_tensor("W", [G, Kd, N], F32, kind="ExternalInput").ap()
    if swiglu:
        W2 = nc.dram_tensor("W2", [G, Kd, N], F32, kind="ExternalInput").ap()
    if rowscale:
        RS = nc.dram_tensor("RS", [G, 128, RT], F32, kind="ExternalInput").ap()
    if bias:
        BI = nc.dram_tensor("BI", [G, 1, N], F32, kind="ExternalInput").ap()
    Y = nc.dram_tensor("Y", [G, R, N], F32, kind="ExternalOutput").ap()
    S = Sch(nc)
    ldq = "pool"
    with contextlib.ExitStack() as st:
        at = _sb(nc, st, [128, KC, R], cdt, "at")
        nwb = 2
        wt = [_sb(nc, st, [128, KC, NT], cdt, "wt") for _ in range(nwb)]
        if swiglu:
            wt2 = [_sb(nc, st, [128, KC, NT], cdt, "wt2") for _ in range(nwb)]
            tmp = [_sb(nc, st, [128, NT], F32, "tmp") for _ in range(2)]
        if rowscale:
            rs = _sb(nc, st, [128, RT], F32, "rs")
        if bias:
            bi = _sb(nc, st, [1, N], F32, "bi")
            ones = _sb(nc, st, [1, 128], F32, "ones")
            S.op("dve", lambda e: e.memset(ones[:], 1.0), writes=["ones"])
        og = [_sb(nc, st, [128, NT], F32, "og") for _ in range(4)]
        npb = 2 if swiglu else 4
        pt = [_ps(nc, st, [128, NT], F32, "pt") for _ in range(npb)]
        if swiglu:
            pt2 = [_ps(nc, st, [128, NT], F32, "pt2") for _ in range(npb)]
        it = 0
        wi = 0
        ATK = ["at_%d" % k0 for k0 in range(0, KC, 8)]
        for g in range(G):
            for k0 in range(0, KC, 8):
                k1 = min(KC, k0 + 8)
                S.dma(ldq, at[:, k0:k1, :],
                      AT[g, k0 * 128:k1 * 128, :].rearrange("(kc p) r -> p kc r", p=128),
                      writes=["at_%d" % k0])
            if silu_a:
                S.op("act", lambda e: e.activation(out=at[:], in_=at[:], func=AF.Silu),
                     reads=ATK, writes=ATK)
            if rowscale:
                S.dma("sp", rs[:], RS[g], writes=["rs"])
            if bias:
                S.dma("sp", bi[:], BI[g], writes=["bi"])
            for nt in range(NTn):
                n0 = nt * NT
                nw = min(NT, N - n0)
                wb = wi % nwb
                wi += 1
                for k0 in range(0, KC, 8):
                    k1 = min(KC, k0 + 8)
                    S.dma(ldq, wt[wb][:, k0:k1, 0:nw],
                          W[g, k0 * 128:k1 * 128, n0:n0 + nw].rearrange("(kc p) n -> p kc n", p=128),
                          writes=["wt%d_%d" % (wb, k0)])
                    if swiglu:
                        S.dma(ldq, wt2[wb][:, k0:k1, 0:nw],
                              W2[g, k0 * 128:k1 * 128, n0:n0 + nw].rearrange("(kc p) n -> p kc n", p=128),
                              writes=["wt2%d_%d" % (wb, k0)])
                for rt in range(RT):
                    r0 = rt * 128
                    rw = min(128, R - r0)
                    pb = it % npb
                    ob = it % 4
                    it += 1

                    def mm(e, pt_=pt[pb], w_=wt[wb], r0=r0, rw=rw, nw=nw, n0=n0):
                        ins = None
                        for kc in range(KC):
                            ins = e.matmul(pt_[0:rw, 0:nw], at[:, kc, r0:r0 + rw], w_[:, kc, 0:nw],
                                           start=(kc == 0), stop=(kc == KC - 1 and not bias))
                        if bias:
                            ins = e.matmul(pt_[0:rw, 0:nw], ones[0:1, 0:rw], bi[0:1, n0:n0 + nw],
                                           start=False, stop=True)
                        return ins
                    rd = ATK + ["wt%d_%d" % (wb, k0) for k0 in range(0, KC, 8)] + (["ones", "bi"] if bias else [])
                    S.op("pe", mm, reads=rd, writes=["pt%d" % pb])
                    if swiglu:
                        def mm2(e, pt_=pt2[pb], w_=wt2[wb], r0=r0, rw=rw, nw=nw):
                            ins = None
                            for kc in range(KC):
                                ins = e.matmul(pt_[0:rw, 0:nw], at[:, kc, r0:r0 + rw], w_[:, kc, 0:nw],
                                               start=(kc == 0), stop=(kc == KC - 1))
                            return ins
                        S.op("pe", mm2, reads=ATK + ["wt2%d_%d" % (wb, k0) for k0 in range(0, KC, 8)], writes=["pt2%d" % pb])
                        tb = it % 2
                        S.op("act", lambda e, t_=tmp[tb], p_=pt[pb], rw=rw, nw=nw:
                             e.activation(out=t_[0:rw, 0:nw], in_=p_[0:rw, 0:nw], func=AF.Silu),
                             reads=["pt%d" % pb], writes=["tmp%d" % tb])
                        S.op("dve", lambda e, o_=og[ob], t_=tmp[tb], p_=pt2[pb], rw=rw, nw=nw:
                             e.tensor_tensor(out=o_[0:rw, 0:nw], in0=t_[0:rw, 0:nw], in1=p_[0:rw, 0:nw], op=ALU.mult),
                             reads=["tmp%d" % tb, "pt2%d" % pb], writes=["og%d" % ob])
                    elif rowscale:
                        S.op("dve", lambda e, o_=og[ob], p_=pt[pb], rw=rw, nw=nw, rt=rt:
                             e.tensor_scalar(out=o_[0:rw, 0:nw], in0=p_[0:rw, 0:nw], scalar1=rs[0:rw, rt:rt + 1],
                                             scalar2=None, op0=ALU.mult),
                             reads=["pt%d" % pb, "rs"], writes=["og%d" % ob])
                    else:
                        if it % 2 == 0:
                            S.op("act", lambda e, o_=og[ob], p_=pt[pb], rw=rw, nw=nw:
                                 e.activation(out=o_[0:rw, 0:nw], in_=p_[0:rw, 0:nw], func=AF.Copy),
                                 reads=["pt%d" % pb], writes=["og%d" % ob])
                        else:
                            S.op("dve", lambda e, o_=og[ob], p_=pt[pb], rw=rw, nw=nw:
                                 e.tensor_copy(out=o_[0:rw, 0:nw], in_=p_[0:rw, 0:nw]),
                                 reads=["pt%d" % pb], writes=["og%d" % ob])
                    S.dma("sp", Y[g, r0:r0 + rw, n0:n0 + nw], og[ob][0:rw, 0:nw],
                          reads=["og%d" % ob], is_output=True)
        S.emit()
    return nc


_prog_cache = {}


def run_prog(key, builder, in_maps):
    import time, sys
    t0 = time.time()
    if key not in _prog_cache:
        _prog_cache[key] = builder()
    nc = _prog_cache[key]
    t1 = time.time()
    res = run_bass_kernel_spmd(nc, in_maps, core_ids=list(range(NCORES)))
    nb = sum(v.nbytes for m in in_maps for v in m.values())
    print("[run_prog] %s build %.1fs run %.1fs in %.0f MB" % (str(key)[:60], t1 - t0, time.time() - t1, nb / 1e6),
          file=sys.stderr, flush=True)
    return res.results


def dev_matmul(ATs, Ws, W2s=None, RSs=None, BIs=None, fp32=False, NT=512, silu_a=False):
    G, Kd, R = ATs[0].shape
    N = Ws[0].shape[2]
    swiglu = W2s is not None
    rowscale = RSs is not None
    bias = BIs is not None
    key = ("mm", G, R, Kd, N, NT, fp32, swiglu, rowscale, bias, silu_a)
    maps = []
    for c in range(NCORES):
        m = {"AT": np.ascontiguousarray(ATs[c], np.float32), "W": np.ascontiguousarray(Ws[c], np.float32)}
        if swiglu:
            m["W2"] = np.ascontiguousarray(W2s[c], np.float32)
        if rowscale:
            m["RS"] = np.ascontiguousarray(RSs[c], np.float32)
        if bias:
            m["BI"] = np.ascontiguousarray(BIs[c], np.float32)
        maps.append(m)
    res = run_prog(key, lambda: build_matmul(G, R, Kd, N, NT=NT, fp32=fp32, swiglu=swiglu,
                                             rowscale=rowscale, bias=bias, silu_a=silu_a), maps)
    return [r["Y"] for r in res]


def build_normmod(R, D, GS, segs, residual, eps=1e-6):
    import contextlib
    nc = bass.Bass("TRN2", target_bir_lowering=False)
    RT = R // 128
    assert R % 128 == 0 and len(segs) == RT
    NSEG = max(segs) + 1
    NG = D // GS
    X = nc.dram_tensor("X", [R, D], F32, kind="ExternalInput").ap()
    VG = nc.dram_tensor("VG", [NSEG, D], F32, kind="ExternalInput").ap()
    VSC = nc.dram_tensor("VSC", [NSEG, D], F32, kind="ExternalInput").ap()
    VSH = nc.dram_tensor("VSH", [NSEG, D], F32, kind="ExternalInput").ap()
    if residual:
        YR = nc.dram_tensor("YR", [R, D], F32, kind="ExternalInput").ap()
        V1 = nc.dram_tensor("V1", [NSEG, D], F32, kind="ExternalInput").ap()
        V2 = nc.dram_tensor("V2", [NSEG, D], F32, kind="ExternalInput").ap()
        XN = nc.dram_tensor("XN", [R, D], F32, kind="ExternalOutput").ap()
    H = nc.dram_tensor("H", [R, D], F32, kind="ExternalOutput").ap()
    S = Sch(nc)
    with contextlib.ExitStack() as st:
        Am = [_sb(nc, st, [128, D], F32, "Am") for _ in range(NSEG)]
        Sh = [_sb(nc, st, [128, D], F32, "Sh") for _ in range(NSEG)]
        if residual:
            V12 = [_sb(nc, st, [128, D], F32, "V12") for _ in range(NSEG)]
        xt = [_sb(nc, st, [128, D], F32, "xt") for _ in range(2)]
        ht = [_sb(nc, st, [128, D], F32, "ht") for _ in range(2)]
        if residual:
            yt = [_sb(nc, st, [128, D], F32, "yt") for _ in range(2)]
        ss = [_sb(nc, st, [128, NG], F32, "ss") for _ in range(2)]
        rr = [_sb(nc, st, [128, NG], F32, "rr") for _ in range(2)]
        for s in range(NSEG):
            S.dma("sp", Am[s][:], VSC[s:s + 1, :].partition_broadcast(128), writes=["Am%d" % s])
            S.dma("sp", Sh[s][:], VG[s:s + 1, :].partition_broadcast(128), writes=["Sh%d" % s])
            S.op("dve", lambda e, a=Am[s], g=Sh[s]: e.scalar_tensor_tensor(
                out=a[:], in0=a[:], scalar=1.0, in1=g[:], op0=ALU.add, op1=ALU.mult),
                reads=["Am%d" % s, "Sh%d" % s], writes=["Am%d" % s])
            S.dma("sp", Sh[s][:], VSH[s:s + 1, :].partition_broadcast(128), reads=[], writes=["Sh%d" % s])
            if residual:
                S.dma("sp", V12[s][:], V1[s:s + 1, :].partition_broadcast(128), writes=["V12%d" % s])
                S.dma("sp", ht[0][:], V2[s:s + 1, :].partition_broadcast(128), writes=["ht0"])
                S.op("dve", lambda e, a=V12[s]: e.tensor_tensor(out=a[:], in0=a[:], in1=ht[0][:], op=ALU.mult),
                     reads=["V12%d" % s, "ht0"], writes=["V12%d" % s])
        for rt in range(RT):
            b = rt % 2
            sg = segs[rt]
            r0 = rt * 128
            xk, hk, yk, sk, rk = "xt%d" % b, "ht%d" % b, "yt%d" % b, "ss%d" % b, "rr%d" % b
            S.dma("sp", xt[b][:], X[r0:r0 + 128, :], writes=[xk])
            if residual:
                S.dma("sp", yt[b][:], YR[r0:r0 + 128, :], writes=[yk])
                S.op("dve", lambda e, b=b, sg=sg: e.tensor_tensor(out=yt[b][:], in0=yt[b][:], in1=V12[sg][:], op=ALU.mult),
                     reads=[yk, "V12%d" % sg], writes=[yk])
                S.op("pool", lambda e, b=b: e.tensor_tensor(out=xt[b][:], in0=xt[b][:], in1=yt[b][:], op=ALU.add),
                     reads=[xk, yk], writes=[xk])
                S.dma("pool", XN[r0:r0 + 128, :], xt[b][:], reads=[xk], is_output=True)
            S.op("act", lambda e, b=b: e.activation(out=ht[b][:], in_=xt[b][:], func=AF.Square),
                 reads=[xk], writes=[hk])
            S.op("dve", lambda e, b=b: e.tensor_reduce(out=ss[b][:], in_=ht[b][:].rearrange("p (g d) -> p g d", d=GS),
                                                        axis=AX.X, op=ALU.add),
                 reads=[hk], writes=[sk])
            S.op("dve", lambda e, b=b: e.tensor_scalar(out=ss[b][:], in0=ss[b][:], scalar1=1.0 / GS, scalar2=eps,
                                                        op0=ALU.mult, op1=ALU.add),
                 reads=[sk], writes=[sk])
            S.op("act", lambda e, b=b: e.activation(out=rr[b][:], in_=ss[b][:], func=AF.Sqrt),
                 reads=[sk], writes=[rk])
            S.op("dve", lambda e, b=b: e.reciprocal(out=rr[b][:], in_=rr[b][:]), reads=[rk], writes=[rk])
            if NG == 1:
                S.op("dve", lambda e, b=b, sg=sg: e.scalar_tensor_tensor(
                    out=ht[b][:], in0=xt[b][:], scalar=rr[b][:, 0:1], in1=Am[sg][:], op0=ALU.mult, op1=ALU.mult),
                    reads=[xk, rk, "Am%d" % sg], writes=[hk])
            else:
                S.op("dve", lambda e, b=b: e.tensor_tensor(
                    out=ht[b][:].rearrange("p (g d) -> p g d", d=GS),
                    in0=xt[b][:].rearrange("p (g d) -> p g d", d=GS),
                    in1=rr[b][:].unsqueeze(2).to_broadcast([128, NG, GS]), op=ALU.mult),
                    reads=[xk, rk], writes=[hk])
                S.op("dve", lambda e, b=b, sg=sg: e.tensor_tensor(out=ht[b][:], in0=ht[b][:], in1=Am[sg][:], op=ALU.mult),
                     reads=[hk, "Am%d" % sg], writes=[hk])
            S.op("pool", lambda e, b=b, sg=sg: e.tensor_tensor(out=ht[b][:], in0=ht[b][:], in1=Sh[sg][:], op=ALU.add),
                 reads=[hk, "Sh%d" % sg], writes=[hk])
            S.dma("pool", H[r0:r0 + 128, :], ht[b][:], reads=[hk], is_output=True)
        S.emit()
    return nc


def dev_normmod(Xs, VG, VSC, VSH, segs, GS, YRs=None, V1=None, V2=None):
    R, D = Xs[0].shape
    residual = YRs is not None
    key = ("nm", R, D, GS, tuple(segs), residual)
    maps = []
    f = lambda a: np.ascontiguousarray(a, np.float32)
    for c in range(NCORES):
        m = {"X": f(Xs[c]), "VG": f(VG[c]), "VSC": f(VSC[c]), "VSH": f(VSH[c])}
        if residual:
            m.update({"YR": f(YRs[c]), "V1": f(V1[c]), "V2": f(V2[c])})
        maps.append(m)
    res = run_prog(key, lambda: build_normmod(R, D, GS, list(segs), residual), maps)
    return [r["H"] for r in res], ([r["XN"] for r in res] if residual else None)


NSLOT = 16
NPOOL = 37


def build_attn():
    import contextlib
    nc = bass.Bass("TRN2", target_bir_lowering=False)
    H, DH = 32, 128
    scale = 1.0 / np.sqrt(DH)
    NKT = NPOOL * 64 + 256
    QT = nc.dram_tensor("QT", [H, DH, NSLOT * 64], F32, kind="ExternalInput").ap()
    KT = nc.dram_tensor("KT", [H, DH, NKT], F32, kind="ExternalInput").ap()
    VA = nc.dram_tensor("VA", [H, 64, NPOOL, 129], F32, kind="ExternalInput").ap()
    VC = nc.dram_tensor("VC", [H, 128, 2, 129], F32, kind="ExternalInput").ap()
    BS = nc.dram_tensor("BS", [H, 64, 3, 512], F32, kind="ExternalInput").ap()
    O = nc.dram_tensor("O", [NSLOT, 64, H * DH], F32, kind="ExternalOutput").ap()
    S = Sch(nc)
    with contextlib.ExitStack() as st:
        qt = [_sb(nc, st, [128, NSLOT * 64], BF16, "qt") for _ in range(2)]
        kt = [_sb(nc, st, [128, NKT], BF16, "kt") for _ in range(2)]
        va = [_sb(nc, st, [64, NPOOL, 129], BF16, "va") for _ in range(2)]
        vc = [_sb(nc, st, [128, 2, 129], BF16, "vc") for _ in range(2)]
        bs = [_sb(nc, st, [64, 3, 512], F32, "bs") for _ in range(2)]
        ost = [_sb(nc, st, [64, NSLOT, DH], F32, "ost") for _ in range(2)]
        sb = [_sb(nc, st, [64, 512], F32, "sb") for _ in range(2)]
        pl = [_sb(nc, st, [64, 512], BF16, "pl") for _ in range(2)]
        pc = [_sb(nc, st, [128, 128], BF16, "pc") for _ in range(2)]
        rc = [_sb(nc, st, [64, 1], F32, "rc") for _ in range(2)]
        psl = [_ps(nc, st, [64, 512], F32, "psl") for _ in range(2)]
        psc = [_ps(nc, st, [128, 128], F32, "psc") for _ in range(2)]
        pso = [_ps(nc, st, [64, 129], F32, "pso") for _ in range(2)]
        it = 0
        for h in range(H):
            hb = h % 2
            S.dma("pool", qt[hb][:], QT[h], writes=["qt%d" % hb])
            S.dma("pool", kt[hb][:], KT[h], writes=["kt%d" % hb])
            S.dma("pool", va[hb][:], VA[h], writes=["va%d" % hb])
            S.dma("pool", vc[hb][:], VC[h], writes=["vc%d" % hb])
            S.dma("sp", bs[hb][:], BS[h], writes=["bs%d" % hb])
            pend = None
            for s in list(range(NSLOT)) + [None]:
                if s is not None:
                    b = it % 2
                    it += 1
                    if s < 14:
                        prow0, typ = s, 0
                    else:
                        prow0, typ = 21 + 8 * (s - 14), s - 13

                    def mm_s(e, b=b, hb=hb, s=s, prow0=prow0):
                        ins = None
                        for j in range(8):
                            ins = e.matmul(psl[b][0:64, j * 64:(j + 1) * 64],
                                           kt[hb][:, (prow0 + j) * 64:(prow0 + j + 1) * 64],
                                           qt[hb][:, s * 64:(s + 1) * 64], start=True, stop=True)
                        return ins
                    S.op("pe", mm_s, reads=["qt%d" % hb, "kt%d" % hb], writes=["psl%d" % b])

                    def mm_c(e, b=b, hb=hb, s=s):
                        ins = None
                        for cb in range(2):
                            ins = e.matmul(psc[b][:, cb * 64:(cb + 1) * 64],
                                           kt[hb][:, NPOOL * 64 + cb * 128:NPOOL * 64 + (cb + 1) * 128],
                                           qt[hb][:, s * 64:(s + 1) * 64], start=True, stop=True)
                        return ins
                    S.op("pe", mm_c, reads=["qt%d" % hb, "kt%d" % hb], writes=["psc%d" % b])
                    S.op("dve", lambda e, b=b, hb=hb, typ=typ: e.scalar_tensor_tensor(
                        out=sb[b][:], in0=psl[b][:], scalar=float(scale), in1=bs[hb][:, typ, :],
                        op0=ALU.mult, op1=ALU.add),
                        reads=["psl%d" % b, "bs%d" % hb], writes=["sb%d" % b])
                    S.op("act", lambda e, b=b: e.activation(out=pl[b][:], in_=sb[b][:], func=AF.Exp),
                         reads=["sb%d" % b], writes=["pl%d" % b])
                    S.op("act", lambda e, b=b: e.activation(out=pc[b][:], in_=psc[b][:], func=AF.Exp, scale=float(scale)),
                         reads=["psc%d" % b], writes=["pc%d" % b])

                    cur = (b, s, prow0)
                else:
                    cur = None
                if pend is not None:
                    b, s, prow0 = pend
                    def mm_o(e, b=b, hb=hb, prow0=prow0):
                        ins = None
                        for j in range(8):
                            ins = e.matmul(pso[b][0:64, 0:129], pl[b][0:64, j * 64:(j + 1) * 64],
                                           va[hb][0:64, prow0 + j, :], start=(j == 0), stop=False)
                        for cb in range(2):
                            ins = e.matmul(pso[b][0:64, 0:129], pc[b][:, cb * 64:(cb + 1) * 64],
                                           vc[hb][:, cb, :], start=False, stop=(cb == 1))
                        return ins
                    S.op("pe", mm_o, reads=["pl%d" % b, "pc%d" % b, "va%d" % hb, "vc%d" % hb], writes=["pso%d" % b])
                    S.op("dve", lambda e, b=b: e.reciprocal(out=rc[b][:], in_=pso[b][0:64, 128:129]),
                         reads=["pso%d" % b], writes=["rc%d" % b])
                    S.op("dve", lambda e, b=b, hb=hb, s=s: e.tensor_scalar(
                        out=ost[hb][:, s, :], in0=pso[b][0:64, 0:128], scalar1=rc[b][:, 0:1], scalar2=None, op0=ALU.mult),
                        reads=["pso%d" % b, "rc%d" % b], writes=["ost%d" % hb])
                pend = cur
            S.dma("sp", O[:, :, h * DH:(h + 1) * DH].rearrange("s q d -> q s d"), ost[hb][:],
                  reads=["ost%d" % hb], is_output=True)
        S.emit()
    return nc


def dev_attn(maps):
    res = run_prog(("attn",), build_attn, maps)
    return [r["O"] for r in res]


def build_softmax(R, E):
    import contextlib
    nc = bass.Bass("TRN2", target_bir_lowering=False)
    T = R // 128
    X = nc.dram_tensor("X", [R, E], F32, kind="ExternalInput").ap()
    Y = nc.dram_tensor("Y", [R, E], F32, kind="ExternalOutput").ap()
    S = Sch(nc)
    with contextlib.ExitStack() as st:
        x = _sb(nc, st, [128, T, E], F32, "x")
        mx = _sb(nc, st, [128, T], F32, "mx")
        S.dma("sp", x[:], X.rearrange("(t p) e -> p t e", p=128), writes=["x"])
        S.op("dve", lambda e: e.tensor_reduce(out=mx[:], in_=x[:], axis=AX.X, op=ALU.max), reads=["x"], writes=["mx"])
        S.op("dve", lambda e: e.tensor_tensor(out=x[:], in0=x[:], in1=mx[:].unsqueeze(2).to_broadcast([128, T, E]),
                                              op=ALU.subtract), reads=["x", "mx"], writes=["x"])
        S.op("act", lambda e: e.activation(out=x[:], in_=x[:], func=AF.Exp), reads=["x"], writes=["x"])
        S.op("dve", lambda e: e.tensor_reduce(out=mx[:], in_=x[:], axis=AX.X, op=ALU.add), reads=["x"], writes=["mx"])
        S.op("dve", lambda e: e.reciprocal(out=mx[:], in_=mx[:]), reads=["mx"], writes=["mx"])
        S.op("dve", lambda e: e.tensor_tensor(out=x[:], in0=x[:], in1=mx[:].unsqueeze(2).to_broadcast([128, T, E]),
                                              op=ALU.mult), reads=["x", "mx"], writes=["x"])
        S.dma("sp", Y.rearrange("(t p) e -> p t e", p=128), x[:], reads=["x"], is_output=True)
        S.emit()
    return nc


def build_thresh(NR, NTOK, CAP, iters=36):
    import contextlib
    nc = bass.Bass("TRN2", target_bir_lowering=False)
    A = nc.dram_tensor("A", [NR, NTOK], F32, kind="ExternalInput").ap()
    M = nc.dram_tensor("M", [NR, NTOK], F32, kind="ExternalOutput").ap()
    S = Sch(nc)
    with contextlib.ExitStack() as st:
        a = _sb(nc, st, [NR, NTOK], F32, "a")
        c = _sb(nc, st, [NR, NTOK], F32, "c")
        lo = _sb(nc, st, [NR, 1], F32, "lo")
        hi = _sb(nc, st, [NR, 1], F32, "hi")
        mid = _sb(nc, st, [NR, 1], F32, "mid")
        cnt = _sb(nc, st, [NR, 1], F32, "cnt")
        d = _sb(nc, st, [NR, 1], F32, "d")
        S.dma("sp", a[:], A, writes=["a"])
        S.op("dve", lambda e: e.memset(lo[:], 0.0), writes=["lo"])
        S.op("dve", lambda e: e.memset(hi[:], 2.0), writes=["hi"])
        for _ in range(iters):
            S.op("dve", lambda e: e.tensor_tensor(out=mid[:], in0=lo[:], in1=hi[:], op=ALU.add),
                 reads=["lo", "hi"], writes=["mid"])
            S.op("dve", lambda e: e.tensor_scalar(out=mid[:], in0=mid[:], scalar1=0.5, scalar2=None, op0=ALU.mult),
                 reads=["mid"], writes=["mid"])
            S.op("dve", lambda e: e.tensor_scalar(out=c[:], in0=a[:], scalar1=mid[:, 0:1], scalar2=None, op0=ALU.is_ge),
                 reads=["a", "mid"], writes=["c"])
            S.op("dve", lambda e: e.tensor_reduce(out=cnt[:], in_=c[:], axis=AX.X, op=ALU.add),
                 reads=["c"], writes=["cnt"])
            S.op("dve", lambda e: e.tensor_scalar(out=cnt[:], in0=cnt[:], scalar1=float(CAP) - 0.5, scalar2=None,
                                                   op0=ALU.is_ge), reads=["cnt"], writes=["cnt"])
            S.op("dve", lambda e: e.tensor_tensor(out=d[:], in0=mid[:], in1=lo[:], op=ALU.subtract),
                 reads=["mid", "lo"], writes=["d"])
            S.op("dve", lambda e: e.scalar_tensor_tensor(out=lo[:], in0=d[:], scalar=cnt[:, 0:1], in1=lo[:],
                                                          op0=ALU.mult, op1=ALU.add),
                 reads=["d", "cnt", "lo"], writes=["lo"])
            S.op("dve", lambda e: e.tensor_tensor(out=d[:], in0=hi[:], in1=mid[:], op=ALU.subtract),
                 reads=["hi", "mid"], writes=["d"])
            S.op("dve", lambda e: e.scalar_tensor_tensor(out=hi[:], in0=d[:], scalar=cnt[:, 0:1], in1=mid[:],
                                                          op0=ALU.mult, op1=ALU.add),
                 reads=["d", "cnt", "mid"], writes=["hi"])
        S.op("dve", lambda e: e.tensor_scalar(out=c[:], in0=a[:], scalar1=lo[:, 0:1], scalar2=None, op0=ALU.is_ge),
             reads=["a", "lo"], writes=["c"])
        S.dma("sp", M, c[:], reads=["c"], is_output=True)
        S.emit()
    return nc


def dev_softmax(Xs):
    R, E = Xs[0].shape
    res = run_prog(("sm", R, E), lambda: build_softmax(R, E),
                   [{"X": np.ascontiguousarray(x, np.float32)} for x in Xs])
    return [r["Y"] for r in res]


def dev_thresh(As, cap):
    NR, NTOK = As[0].shape
    res = run_prog(("th", NR, NTOK, cap), lambda: build_thresh(NR, NTOK, cap),
                   [{"A": np.ascontiguousarray(a, np.float32)} for a in As])
    return [r["M"] for r in res]


D = 4096
NTOKB = 4096
GRID = 64
NH, DHD = 32, 128
NE, CAP, DFF = 16, 512, 1536
LMAX = 2560
LSEG = 512
POOLW = (2, 4, 8, 16)
ATT_INT0 = (4, 18, 32, 46)
ATT_SP = ((0, 1), (2, 3), (60, 61), (62, 63))
_EMU = [False]


def _T(a):
    return np.ascontiguousarray(np.swapaxes(a, -1, -2))


def _attn_rows(q):
    rows = [ATT_INT0[q] + s for s in range(14)] + list(ATT_SP[q])
    pool = [ATT_INT0[q] - 4 + w for w in range(21)]
    for r in ATT_SP[q]:
        r0 = min(max(r - 4, 0), GRID - 8)
        pool += [r0 + j for j in range(8)]
    return rows, pool


def _bias_tables(rpb, q):
    cols = np.arange(GRID)
    cstart = np.clip(cols - 8, 0, GRID - 16)
    valid = (cols[None, :] >= cstart[:, None]) & (cols[None, :] < cstart[:, None] + 16)
    dc = np.clip(cols[None, :] - cols[:, None] + 15, 0, 30)
    out = np.empty((NH, 64, 3, 512), np.float32)
    types = [None] + list(ATT_SP[q])
    for t, r in enumerate(types):
        if r is None:
            dr = np.arange(8) + 3
        else:
            r0 = min(max(r - 4, 0), GRID - 8)
            dr = r0 + np.arange(8) - r + 7
        B = rpb[:, dr[:, None, None], dc[None, :, :]]
        B = np.where(valid[None, None], B, np.float32(-30000.0))
        out[:, :, t, :] = B.transpose(0, 3, 1, 2).reshape(NH, 64, 512)
    return out


def _moe(hf, x_in, gvec, l, inp):
    c8 = range(NCORES)
    logits = dev_matmul([_T(hf[1024 * c:1024 * c + 1024])[None] for c in c8],
                        [inp["moe_w_router"][l][None]] * NCORES, fp32=True, NT=16)
    aff = np.concatenate(dev_softmax([y[0] for y in logits]), 0)
    AFT = aff.reshape(2, NTOKB, NE).transpose(0, 2, 1).reshape(2 * NE, NTOKB)
    mask = np.concatenate(dev_thresh([AFT[4 * c:4 * c + 4] for c in c8], CAP), 0)
    idx = np.zeros((2, NE, CAP), np.int64)
    for b in range(2):
        for e in range(NE):
            nz = np.nonzero(mask[b * NE + e] > 0.5)[0]
            idx[b, e, :min(CAP, len(nz))] = nz[:CAP]
    gidx = idx + (np.arange(2) * NTOKB)[:, None, None]
    ATs, RSs = [], []
    for c in c8:
        a = np.stack([np.concatenate([hf[gidx[0, e]], hf[gidx[1, e]]], 0) for e in (2 * c, 2 * c + 1)])
        ATs.append(_T(a))
        g = np.stack([np.concatenate([aff[gidx[0, e], e], aff[gidx[1, e], e]]) for e in (2 * c, 2 * c + 1)])
        RSs.append(g.reshape(2, 8, 128).transpose(0, 2, 1))
    hid = dev_matmul(ATs, [inp["moe_w_gate"][l][2 * c:2 * c + 2] for c in c8],
                     W2s=[inp["moe_w_up"][l][2 * c:2 * c + 2] for c in c8], NT=256)
    y = dev_matmul([_T(h) for h in hid], [inp["moe_w_down"][l][2 * c:2 * c + 2] for c in c8], RSs=RSs)
    yall = np.stack(y).reshape(NE, 2, CAP, D)
    ATc, Wc = [], []
    for c in c8:
        b, t0 = c // 4, (c % 4) * 1024
        yl = np.zeros((8, LSEG, D), np.float32)
        sel = np.zeros((8, LSEG, 128), np.float32)
        for tt in range(8):
            lo = t0 + 128 * tt
            ee, ss = np.nonzero((idx[b] >= lo) & (idx[b] < lo + 128))
            n = min(len(ee), LSEG)
            ee, ss = ee[:n], ss[:n]
            yl[tt, :n] = yall[ee, b, ss]
            sel[tt, np.arange(n), idx[b, ee, ss] - lo] = 1.0
        ATc.append(sel)
        Wc.append(yl)
    out = dev_matmul(ATc, Wc, fp32=True)
    return np.concatenate([o.reshape(1024, D) for o in out], 0)


def kernel(x, c, ctx, c_ctx, ada_w, ada_b, norm_mix_g, norm_ffn_g, na_w_qkv, na_q_gain, na_k_gain,
           na_rpb, na_w_out, pool_w, pool_scale, moe_w_router, moe_w_gate, moe_w_up, moe_w_down):
    inp = dict(moe_w_router=np.asarray(moe_w_router), moe_w_gate=np.asarray(moe_w_gate),
               moe_w_up=np.asarray(moe_w_up), moe_w_down=np.asarray(moe_w_down))
    f = lambda a: np.asarray(a, np.float32)
    x = f(x).reshape(2 * NTOKB, D)
    ctxf = f(ctx).reshape(512, D)
    c8 = range(NCORES)
    ones = np.ones(D, np.float32)
    zeros = np.zeros(D, np.float32)
    cond = np.stack([f(c)[0], f(c)[1], f(c_ctx)])
    ATa = np.stack([cond.T, cond.T])
    ada_w = np.asarray(ada_w)
    ada_b = f(ada_b)
    Ya = dev_matmul([ATa] * NCORES, [ada_w[:, :, 3072 * k:3072 * (k + 1)] for k in c8],
                    BIs=[ada_b[:, None, 3072 * k:3072 * (k + 1)] for k in c8], fp32=True, silu_a=True)
    mods = np.concatenate(Ya, -1).reshape(2, 3, 6, D)
    g_mix, g_ffn = f(norm_mix_g), f(norm_ffn_g)

    m0 = mods[0]
    Xs, VG, VSC, VSH = [], [], [], []
    for k in c8:
        b = k // 4
        Xs.append(np.concatenate([x[1024 * k:1024 * k + 1024], ctxf[64 * k:64 * k + 64], np.zeros((64, D), np.float32)]))
        VG.append(np.stack([g_mix[0], g_mix[0]]))
        VSC.append(np.stack([m0[b, 1], m0[2, 1]]))
        VSH.append(np.stack([m0[b, 0], m0[2, 0]]))
    Hh, _ = dev_normmod(Xs, VG, VSC, VSH, [0] * 8 + [1], D)
    Wq = np.asarray(na_w_qkv)[0][None]
    qkv = dev_matmul([_T(h)[None] for h in Hh], [Wq] * NCORES)
    qkv_lat = np.concatenate([y[0][:1024] for y in qkv], 0)
    qkv_ctx = np.concatenate([y[0][1024:1088] for y in qkv], 0)
    qg, kg = np.tile(f(na_q_gain)[0], NH), np.tile(f(na_k_gain)[0], NH)
    Xs = []
    for k in c8:
        sl = slice(1024 * k, 1024 * k + 1024)
        Xs.append(np.concatenate([qkv_lat[sl, 0:D], qkv_lat[sl, D:2 * D], qkv_ctx[64 * k:64 * k + 64, D:2 * D],
                                  np.zeros((64, D), np.float32)]))
    Hn, _ = dev_normmod(Xs, [np.stack([qg, kg])] * NCORES, [np.stack([zeros, zeros])] * NCORES,
                        [np.stack([zeros, zeros])] * NCORES, [0] * 8 + [1] * 9, DHD)
    qn = np.concatenate([h[0:1024] for h in Hn], 0)
    kn = np.concatenate([h[1024:2048] for h in Hn], 0)
    kcn = np.concatenate([h[2048:2112] for h in Hn], 0)
    vl = qkv_lat[:, 2 * D:3 * D]
    vcx = qkv_ctx[:, 2 * D:3 * D]
    rpb = f(na_rpb)[0]
    maps = []
    for k in c8:
        b, q = k // 4, k % 4
        rows, pool = _attn_rows(q)
        gq = qn[b * NTOKB:(b + 1) * NTOKB].reshape(GRID, GRID, NH, DHD)
        gk = kn[b * NTOKB:(b + 1) * NTOKB].reshape(GRID, GRID, NH, DHD)
        gv = vl[b * NTOKB:(b + 1) * NTOKB].reshape(GRID, GRID, NH, DHD)
        QT = gq[rows].transpose(2, 3, 0, 1).reshape(NH, DHD, NSLOT * 64)
        KTl = gk[pool].transpose(2, 3, 0, 1).reshape(NH, DHD, NPOOL * 64)
        KTc = kcn[256 * b:256 * b + 256].reshape(256, NH, DHD).transpose(1, 2, 0)
        VA = np.ones((NH, 64, NPOOL, 129), np.float32)
        VA[..., :128] = gv[pool].transpose(2, 1, 0, 3)
        VC = np.ones((NH, 128, 2, 129), np.float32)
        VC[..., :128] = vcx[256 * b:256 * b + 256].reshape(2, 128, NH, DHD).transpose(2, 1, 0, 3)
        maps.append({"QT": np.ascontiguousarray(QT), "KT": np.ascontiguousarray(np.concatenate([KTl, KTc], -1)),
                     "VA": VA, "VC": VC, "BS": _bias_tables(rpb, q)})
    Oc = dev_attn(maps)
    o_all = np.zeros((2, GRID, GRID, D), np.float32)
    for k in c8:
        rows, _ = _attn_rows(k % 4)
        o_all[k // 4, rows] = Oc[k]
    o_all = o_all.reshape(2 * NTOKB, D)
    Wo = np.asarray(na_w_out)[0][None]
    ya = dev_matmul([_T(o_all[1024 * k:1024 * k + 1024])[None] for k in c8], [Wo] * NCORES)
    hf, x1 = dev_normmod([x[1024 * k:1024 * k + 1024] for k in c8],
                         [g_ffn[0][None]] * NCORES, [m0[k // 4, 4][None] for k in c8], [m0[k // 4, 3][None] for k in c8],
                         [0] * 8, D, YRs=[y[0] for y in ya], V1=[m0[k // 4, 2][None] for k in c8], V2=[ones[None]] * NCORES)
    hf, x1 = np.concatenate(hf, 0), np.concatenate(x1, 0)
    mo = _moe(hf, x1, None, 0, inp)

    m1 = mods[1]
    h1, x2 = dev_normmod([x1[1024 * k:1024 * k + 1024] for k in c8],
                         [g_mix[1][None]] * NCORES, [m1[k // 4, 1][None] for k in c8], [m1[k // 4, 0][None] for k in c8],
                         [0] * 8, D, YRs=[mo[1024 * k:1024 * k + 1024] for k in c8],
                         V1=[m0[k // 4, 5][None] for k in c8], V2=[ones[None]] * NCORES)
    h1, x2 = np.concatenate(h1, 0), np.concatenate(x2, 0)
    ATp, Wp = [], []
    for k in c8:
        b, n0 = k // 4, (k % 4) * 1024
        pos = np.arange(n0 - 64, n0 + 1088)
        inb = (pos >= 0) & (pos < NTOKB)
        hh = np.zeros((1152, D), np.float32)
        hh[inb] = h1[b * NTOKB + pos[inb]]
        A = np.zeros((4, 1152, 1024), np.float32)
        n = n0 + np.arange(1024)
        for g, w in enumerate(POOLW):
            lo = np.clip(n - w // 2, 0, NTOKB)
            hi = np.clip(n + w // 2, 0, NTOKB)
            inv = (1.0 / (hi - lo)).astype(np.float32)
            for t in range(1024):
                A[g, lo[t] - (n0 - 64):hi[t] - (n0 - 64), t] = inv[t]
                A[g, n[t] - (n0 - 64), t] -= 1.0
        ATp.append(A)
        Wp.append(np.ascontiguousarray(hh.reshape(1152, 4, 1024).transpose(1, 0, 2)))
    pooled = dev_matmul(ATp, Wp, fp32=True)
    yp = dev_matmul([_T(p) for p in pooled], [np.asarray(pool_w)[0]] * NCORES)
    yp = [np.ascontiguousarray(y.transpose(1, 0, 2).reshape(1024, D)) for y in yp]
    ps = f(pool_scale)[0]
    hf, x3 = dev_normmod([x2[1024 * k:1024 * k + 1024] for k in c8],
                         [g_ffn[1][None]] * NCORES, [m1[k // 4, 4][None] for k in c8], [m1[k // 4, 3][None] for k in c8],
                         [0] * 8, D, YRs=yp, V1=[m1[k // 4, 2][None] for k in c8], V2=[ps[None]] * NCORES)
    hf, x3 = np.concatenate(hf, 0), np.concatenate(x3, 0)
    mo = _moe(hf, x3, None, 1, inp)
    _, x4 = dev_normmod([x3[1024 * k:1024 * k + 1024] for k in c8],
                        [ones[None]] * NCORES, [zeros[None]] * NCORES, [zeros[None]] * NCORES,
                        [0] * 8, D, YRs=[mo[1024 * k:1024 * k + 1024] for k in c8],
                        V1=[m1[k // 4, 5][None] for k in c8], V2=[ones[None]] * NCORES)
    return np.concatenate(x4, 0).reshape(2, NTOKB, D).astype(np.float32)
```

```python
import numpy as np
import concourse.bass as bass
import concourse.mybir as mybir
from concourse.bass_utils import run_bass_kernel_spmd

F32 = mybir.dt.float32
BF16 = mybir.dt.bfloat16
AF = mybir.ActivationFunctionType
ALU = mybir.AluOpType
AX = mybir.AxisListType
NCORES = 8
ENGS = ("pe", "act", "dve", "pool", "sp")
NDS = 24


class Sch:
    def __init__(self, nc):
        self.nc = nc
        self.prog = {e: [] for e in ENGS}
        self.cnt = {e: 0 for e in ENGS}
        self.waited = {e: {} for e in ENGS}
        self.last_w = {}
        self.readers = {}
        self.dnext = 0
        self.dval = [0] * NDS
        self.out_events = []

    def _deps(self, eng, reads, writes):
        deps = []
        for r in reads:
            if r in self.last_w:
                deps.append(self.last_w[r])
        for w in writes:
            if w in self.last_w:
                deps.append(self.last_w[w])
            deps.extend(self.readers.get(w, []))
        need = {}
        for (s, v) in deps:
            if s == eng and eng == "pe":
                continue
            if v > need.get(s, 0):
                need[s] = v
        out = []
        for s, v in need.items():
            if self.waited[eng].get(s, 0) < v:
                self.waited[eng][s] = v
                out.append((s, v))
        return out

    def _commit(self, ev, reads, writes):
        for w in writes:
            self.last_w[w] = ev
            self.readers[w] = []
        for r in reads:
            if r not in writes:
                self.readers.setdefault(r, []).append(ev)

    def op(self, eng, fn, reads=(), writes=()):
        waits = self._deps(eng, reads, writes)
        self.cnt[eng] += 1
        ev = (eng, self.cnt[eng])
        self.prog[eng].append((waits, fn, eng, 1))
        self._commit(ev, reads, writes)
        return ev

    def dma(self, eng, out, in_, reads=(), writes=(), is_output=False):
        waits = self._deps(eng, reads, writes)
        j = self.dnext
        self.dnext = (self.dnext + 1) % NDS
        s = "d%d" % j
        if self.dval[j] > 0 and self.waited[eng].get(s, 0) < self.dval[j]:
            self.waited[eng][s] = self.dval[j]
            waits.append((s, self.dval[j]))
        self.dval[j] += 16
        ev = (s, self.dval[j])
        self.prog[eng].append((waits, lambda e, o=out, i=in_: e.dma_start(out=o, in_=i), s, 16))
        self._commit(ev, reads, writes)
        if is_output:
            self.out_events.append(ev)
        return ev

    def emit(self):
        nc = self.nc
        import contextlib
        with contextlib.ExitStack() as st:
            sems = {}
            for e in ENGS:
                sems[e] = st.enter_context(nc.semaphore("s_" + e))
            for j in range(NDS):
                sems["d%d" % j] = st.enter_context(nc.semaphore("s_d%d" % j))
            fin = {}
            for (s, v) in self.out_events:
                fin[s] = max(fin.get(s, 0), v)
            block = st.enter_context(nc.Block())

            def run(engobj, name):
                for (waits, fn, semname, inc) in self.prog[name]:
                    for (s, v) in waits:
                        engobj.wait_ge(sems[s], v)
                    fn(engobj).then_inc(sems[semname], inc)
                if name == "sp":
                    for s, v in fin.items():
                        engobj.wait_ge(sems[s], v)

            @block.tensor
            def _(e):
                run(e, "pe")

            @block.scalar
            def _(e):
                run(e, "act")

            @block.vector
            def _(e):
                run(e, "dve")

            @block.gpsimd
            def _(e):
                run(e, "pool")

            @block.sync
            def _(e):
                run(e, "sp")


_uid = [0]


def _sb(nc, st, shape, dt, name):
    _uid[0] += 1
    return st.enter_context(nc.sbuf_tensor("%s_%d" % (name, _uid[0]), shape, dt))


def _ps(nc, st, shape, dt, name):
    _uid[0] += 1
    return st.enter_context(nc.psum_tensor("%s_%d" % (name, _uid[0]), shape, dt))


def build_matmul(G, R, Kd, N, NT=512, fp32=False, swiglu=False, rowscale=False,
                 bias=False, silu_a=False, band=None):
    import contextlib
    nc = bass.Bass("TRN2", target_bir_lowering=False)
    KC = Kd // 128
    assert Kd % 128 == 0
    RT = (R + 127) // 128
    NTn = (N + NT - 1) // NT
    cdt = F32 if fp32 else BF16
    AT = nc.dram_tensor("AT", [G, Kd, R], F32, kind="ExternalInput").ap()
    W = nc.dram_tensor("W", [G, Kd, N], F32, kind="ExternalInput").ap()
    if swiglu:
        W2 = nc.dram_tensor("W2", [G, Kd, N], F32, kind="ExternalInput").ap()
    if rowscale:
        RS = nc.dram_tensor("RS", [G, 128, RT], F32, kind="ExternalInput").ap()
    if bias:
        BI = nc.dram_tensor("BI", [G, 1, N], F32, kind="ExternalInput").ap()
    Y = nc.dram_tensor("Y", [G, R, N], F32, kind="ExternalOutput").ap()
    S = Sch(nc)
    ldq = "pool"
    with contextlib.ExitStack() as st:
        at = _sb(nc, st, [128, KC, R], cdt, "at")
        nwb = 2
        wt = [_sb(nc, st, [128, KC, NT], cdt, "wt") for _ in range(nwb)]
        if swiglu:
            wt2 = [_sb(nc, st, [128, KC, NT], cdt, "wt2") for _ in range(nwb)]
            tmp = [_sb(nc, st, [128, NT], F32, "tmp") for _ in range(2)]
        if rowscale:
            rs = _sb(nc, st, [128, RT], F32, "rs")
        if bias:
            bi = _sb(nc, st, [1, N], F32, "bi")
            ones = _sb(nc, st, [1, 128], F32, "ones")
            S.op("dve", lambda e: e.memset(ones[:], 1.0), writes=["ones"])
        og = [_sb(nc, st, [128, NT], F32, "og") for _ in range(4)]
        npb = 2 if swiglu else 4
        pt = [_ps(nc, st, [128, NT], F32, "pt") for _ in range(npb)]
        if swiglu:
            pt2 = [_ps(nc, st, [128, NT], F32, "pt2") for _ in range(npb)]
        it = 0
        wi = 0
        ATK = ["at_%d" % k0 for k0 in range(0, KC, 8)]
        for g in range(G):
            for k0 in range(0, KC, 8):
                k1 = min(KC, k0 + 8)
                S.dma(ldq, at[:, k0:k1, :],
                      AT[g, k0 * 128:k1 * 128, :].rearrange("(kc p) r -> p kc r", p=128),
                      writes=["at_%d" % k0])
            if silu_a:
                S.op("act", lambda e: e.activation(out=at[:], in_=at[:], func=AF.Silu),
                     reads=ATK, writes=ATK)
            if rowscale:
                S.dma("sp", rs[:], RS[g], writes=["rs"])
            if bias:
                S.dma("sp", bi[:], BI[g], writes=["bi"])
            for nt in range(NTn):
                n0 = nt * NT
                nw = min(NT, N - n0)
                wb = wi % nwb
                wi += 1
                for k0 in range(0, KC, 8):
                    k1 = min(KC, k0 + 8)
                    S.dma(ldq, wt[wb][:, k0:k1, 0:nw],
                          W[g, k0 * 128:k1 * 128, n0:n0 + nw].rearrange("(kc p) n -> p kc n", p=128),
                          writes=["wt%d_%d" % (wb, k0)])
                    if swiglu:
                        S.dma(ldq, wt2[wb][:, k0:k1, 0:nw],
                              W2[g, k0 * 128:k1 * 128, n0:n0 + nw].rearrange("(kc p) n -> p kc n", p=128),
                              writes=["wt2%d_%d" % (wb, k0)])
                for rt in range(RT):
                    r0 = rt * 128
                    rw = min(128, R - r0)
                    pb = it % npb
                    ob = it % 4
                    it += 1

                    kcs = list(range(KC)) if band is None else list(range(max(0, rt + band[0]), min(KC, rt + band[1] + 1)))

                    def mm(e, pt_=pt[pb], w_=wt[wb], r0=r0, rw=rw, nw=nw, n0=n0, kcs=kcs):
                        ins = None
                        for kc in kcs:
                            ins = e.matmul(pt_[0:rw, 0:nw], at[:, kc, r0:r0 + rw], w_[:, kc, 0:nw],
                                           start=(kc == kcs[0]), stop=(kc == kcs[-1] and not bias))
                        if bias:
                            ins = e.matmul(pt_[0:rw, 0:nw], ones[0:1, 0:rw], bi[0:1, n0:n0 + nw],
                                           start=False, stop=True)
                        return ins
                    rd = ATK + ["wt%d_%d" % (wb, k0) for k0 in range(0, KC, 8)] + (["ones", "bi"] if bias else [])
                    S.op("pe", mm, reads=rd, writes=["pt%d" % pb])
                    if swiglu:
                        def mm2(e, pt_=pt2[pb], w_=wt2[wb], r0=r0, rw=rw, nw=nw):
                            ins = None
                            for kc in range(KC):
                                ins = e.matmul(pt_[0:rw, 0:nw], at[:, kc, r0:r0 + rw], w_[:, kc, 0:nw],
                                               start=(kc == 0), stop=(kc == KC - 1))
                            return ins
                        S.op("pe", mm2, reads=ATK + ["wt2%d_%d" % (wb, k0) for k0 in range(0, KC, 8)], writes=["pt2%d" % pb])
                        tb = it % 2
                        S.op("act", lambda e, t_=tmp[tb], p_=pt[pb], rw=rw, nw=nw:
                             e.activation(out=t_[0:rw, 0:nw], in_=p_[0:rw, 0:nw], func=AF.Silu),
                             reads=["pt%d" % pb], writes=["tmp%d" % tb])
                        S.op("dve", lambda e, o_=og[ob], t_=tmp[tb], p_=pt2[pb], rw=rw, nw=nw:
                             e.tensor_tensor(out=o_[0:rw, 0:nw], in0=t_[0:rw, 0:nw], in1=p_[0:rw, 0:nw], op=ALU.mult),
                             reads=["tmp%d" % tb, "pt2%d" % pb], writes=["og%d" % ob])
                    elif rowscale:
                        S.op("dve", lambda e, o_=og[ob], p_=pt[pb], rw=rw, nw=nw, rt=rt:
                             e.tensor_scalar(out=o_[0:rw, 0:nw], in0=p_[0:rw, 0:nw], scalar1=rs[0:rw, rt:rt + 1],
                                             scalar2=None, op0=ALU.mult),
                             reads=["pt%d" % pb, "rs"], writes=["og%d" % ob])
                    else:
                        if it % 2 == 0:
                            S.op("act", lambda e, o_=og[ob], p_=pt[pb], rw=rw, nw=nw:
                                 e.activation(out=o_[0:rw, 0:nw], in_=p_[0:rw, 0:nw], func=AF.Copy),
                                 reads=["pt%d" % pb], writes=["og%d" % ob])
                        else:
                            S.op("dve", lambda e, o_=og[ob], p_=pt[pb], rw=rw, nw=nw:
                                 e.tensor_copy(out=o_[0:rw, 0:nw], in_=p_[0:rw, 0:nw]),
                                 reads=["pt%d" % pb], writes=["og%d" % ob])
                    S.dma("sp", Y[g, r0:r0 + rw, n0:n0 + nw], og[ob][0:rw, 0:nw],
                          reads=["og%d" % ob], is_output=True)
        S.emit()
    return nc


_prog_cache = {}


def run_prog(key, builder, in_maps):
    import time, sys
    t0 = time.time()
    if key not in _prog_cache:
        _prog_cache[key] = builder()
    nc = _prog_cache[key]
    t1 = time.time()
    res = run_bass_kernel_spmd(nc, in_maps, core_ids=list(range(NCORES)))
    nb = sum(v.nbytes for m in in_maps for v in m.values())
    print("[run_prog] %s build %.1fs run %.1fs in %.0f MB" % (str(key)[:60], t1 - t0, time.time() - t1, nb / 1e6),
          file=sys.stderr, flush=True)
    return res.results


def dev_matmul(ATs, Ws, W2s=None, RSs=None, BIs=None, fp32=False, NT=512, silu_a=False, band=None):
    G, Kd, R = ATs[0].shape
    N = Ws[0].shape[2]
    swiglu = W2s is not None
    rowscale = RSs is not None
    bias = BIs is not None
    key = ("mm", G, R, Kd, N, NT, fp32, swiglu, rowscale, bias, silu_a, band)
    maps = []
    for c in range(NCORES):
        m = {"AT": np.ascontiguousarray(ATs[c], np.float32), "W": np.ascontiguousarray(Ws[c], np.float32)}
        if swiglu:
            m["W2"] = np.ascontiguousarray(W2s[c], np.float32)
        if rowscale:
            m["RS"] = np.ascontiguousarray(RSs[c], np.float32)
        if bias:
            m["BI"] = np.ascontiguousarray(BIs[c], np.float32)
        maps.append(m)
    res = run_prog(key, lambda: build_matmul(G, R, Kd, N, NT=NT, fp32=fp32, swiglu=swiglu,
                                             rowscale=rowscale, bias=bias, silu_a=silu_a, band=band), maps)
    return [r["Y"] for r in res]


def build_normmod(R, D, GS, segs, residual, eps=1e-6):
    import contextlib
    nc = bass.Bass("TRN2", target_bir_lowering=False)
    RT = R // 128
    assert R % 128 == 0 and len(segs) == RT
    NSEG = max(segs) + 1
    NG = D // GS
    X = nc.dram_tensor("X", [R, D], F32, kind="ExternalInput").ap()
    VG = nc.dram_tensor("VG", [NSEG, D], F32, kind="ExternalInput").ap()
    VSC = nc.dram_tensor("VSC", [NSEG, D], F32, kind="ExternalInput").ap()
    VSH = nc.dram_tensor("VSH", [NSEG, D], F32, kind="ExternalInput").ap()
    if residual:
        YR = nc.dram_tensor("YR", [R, D], F32, kind="ExternalInput").ap()
        V1 = nc.dram_tensor("V1", [NSEG, D], F32, kind="ExternalInput").ap()
        V2 = nc.dram_tensor("V2", [NSEG, D], F32, kind="ExternalInput").ap()
        XN = nc.dram_tensor("XN", [R, D], F32, kind="ExternalOutput").ap()
    H = nc.dram_tensor("H", [R, D], F32, kind="ExternalOutput").ap()
    S = Sch(nc)
    with contextlib.ExitStack() as st:
        Am = [_sb(nc, st, [128, D], F32, "Am") for _ in range(NSEG)]
        Sh = [_sb(nc, st, [128, D], F32, "Sh") for _ in range(NSEG)]
        if residual:
            V12 = [_sb(nc, st, [128, D], F32, "V12") for _ in range(NSEG)]
        xt = [_sb(nc, st, [128, D], F32, "xt") for _ in range(2)]
        ht = [_sb(nc, st, [128, D], F32, "ht") for _ in range(2)]
        if residual:
            yt = [_sb(nc, st, [128, D], F32, "yt") for _ in range(2)]
        ss = [_sb(nc, st, [128, NG], F32, "ss") for _ in range(2)]
        rr = [_sb(nc, st, [128, NG], F32, "rr") for _ in range(2)]
        for s in range(NSEG):
            S.dma("sp", Am[s][:], VSC[s:s + 1, :].partition_broadcast(128), writes=["Am%d" % s])
            S.dma("sp", Sh[s][:], VG[s:s + 1, :].partition_broadcast(128), writes=["Sh%d" % s])
            S.op("dve", lambda e, a=Am[s], g=Sh[s]: e.scalar_tensor_tensor(
                out=a[:], in0=a[:], scalar=1.0, in1=g[:], op0=ALU.add, op1=ALU.mult),
                reads=["Am%d" % s, "Sh%d" % s], writes=["Am%d" % s])
            S.dma("sp", Sh[s][:], VSH[s:s + 1, :].partition_broadcast(128), reads=[], writes=["Sh%d" % s])
            if residual:
                S.dma("sp", V12[s][:], V1[s:s + 1, :].partition_broadcast(128), writes=["V12%d" % s])
                S.dma("sp", ht[0][:], V2[s:s + 1, :].partition_broadcast(128), writes=["ht0"])
                S.op("dve", lambda e, a=V12[s]: e.tensor_tensor(out=a[:], in0=a[:], in1=ht[0][:], op=ALU.mult),
                     reads=["V12%d" % s, "ht0"], writes=["V12%d" % s])
        for rt in range(RT):
            b = rt % 2
            sg = segs[rt]
            r0 = rt * 128
            xk, hk, yk, sk, rk = "xt%d" % b, "ht%d" % b, "yt%d" % b, "ss%d" % b, "rr%d" % b
            S.dma("sp", xt[b][:], X[r0:r0 + 128, :], writes=[xk])
            if residual:
                S.dma("sp", yt[b][:], YR[r0:r0 + 128, :], writes=[yk])
                S.op("dve", lambda e, b=b, sg=sg: e.tensor_tensor(out=yt[b][:], in0=yt[b][:], in1=V12[sg][:], op=ALU.mult),
                     reads=[yk, "V12%d" % sg], writes=[yk])
                S.op("pool", lambda e, b=b: e.tensor_tensor(out=xt[b][:], in0=xt[b][:], in1=yt[b][:], op=ALU.add),
                     reads=[xk, yk], writes=[xk])
                S.dma("pool", XN[r0:r0 + 128, :], xt[b][:], reads=[xk], is_output=True)
            S.op("act", lambda e, b=b: e.activation(out=ht[b][:], in_=xt[b][:], func=AF.Square),
                 reads=[xk], writes=[hk])
            S.op("dve", lambda e, b=b: e.tensor_reduce(out=ss[b][:], in_=ht[b][:].rearrange("p (g d) -> p g d", d=GS),
                                                        axis=AX.X, op=ALU.add),
                 reads=[hk], writes=[sk])
            S.op("dve", lambda e, b=b: e.tensor_scalar(out=ss[b][:], in0=ss[b][:], scalar1=1.0 / GS, scalar2=eps,
                                                        op0=ALU.mult, op1=ALU.add),
                 reads=[sk], writes=[sk])
            S.op("act", lambda e, b=b: e.activation(out=rr[b][:], in_=ss[b][:], func=AF.Sqrt),
                 reads=[sk], writes=[rk])
            S.op("dve", lambda e, b=b: e.reciprocal(out=rr[b][:], in_=rr[b][:]), reads=[rk], writes=[rk])
            if NG == 1:
                S.op("dve", lambda e, b=b, sg=sg: e.scalar_tensor_tensor(
                    out=ht[b][:], in0=xt[b][:], scalar=rr[b][:, 0:1], in1=Am[sg][:], op0=ALU.mult, op1=ALU.mult),
                    reads=[xk, rk, "Am%d" % sg], writes=[hk])
            else:
                S.op("dve", lambda e, b=b: e.tensor_tensor(
                    out=ht[b][:].rearrange("p (g d) -> p g d", d=GS),
                    in0=xt[b][:].rearrange("p (g d) -> p g d", d=GS),
                    in1=rr[b][:].unsqueeze(2).to_broadcast([128, NG, GS]), op=ALU.mult),
                    reads=[xk, rk], writes=[hk])
                S.op("dve", lambda e, b=b, sg=sg: e.tensor_tensor(out=ht[b][:], in0=ht[b][:], in1=Am[sg][:], op=ALU.mult),
                     reads=[hk, "Am%d" % sg], writes=[hk])
            S.op("pool", lambda e, b=b, sg=sg: e.tensor_tensor(out=ht[b][:], in0=ht[b][:], in1=Sh[sg][:], op=ALU.add),
                 reads=[hk, "Sh%d" % sg], writes=[hk])
            S.dma("pool", H[r0:r0 + 128, :], ht[b][:], reads=[hk], is_output=True)
        S.emit()
    return nc


def dev_normmod(Xs, VG, VSC, VSH, segs, GS, YRs=None, V1=None, V2=None):
    R, D = Xs[0].shape
    residual = YRs is not None
    key = ("nm", R, D, GS, tuple(segs), residual)
    maps = []
    f = lambda a: np.ascontiguousarray(a, np.float32)
    for c in range(NCORES):
        m = {"X": f(Xs[c]), "VG": f(VG[c]), "VSC": f(VSC[c]), "VSH": f(VSH[c])}
        if residual:
            m.update({"YR": f(YRs[c]), "V1": f(V1[c]), "V2": f(V2[c])})
        maps.append(m)
    res = run_prog(key, lambda: build_normmod(R, D, GS, list(segs), residual), maps)
    return [r["H"] for r in res], ([r["XN"] for r in res] if residual else None)


NSLOT = 16
NPOOL = 37


def build_attn():
    import contextlib
    nc = bass.Bass("TRN2", target_bir_lowering=False)
    H, DH = 32, 128
    scale = 1.0 / np.sqrt(DH)
    NKT = NPOOL * 64 + 256
    QT = nc.dram_tensor("QT", [H, DH, NSLOT * 64], F32, kind="ExternalInput").ap()
    KT = nc.dram_tensor("KT", [H, DH, NKT], F32, kind="ExternalInput").ap()
    VA = nc.dram_tensor("VA", [H, 64, NPOOL, 129], F32, kind="ExternalInput").ap()
    VC = nc.dram_tensor("VC", [H, 128, 2, 129], F32, kind="ExternalInput").ap()
    BS = nc.dram_tensor("BS", [H, 64, 3, 512], F32, kind="ExternalInput").ap()
    O = nc.dram_tensor("O", [NSLOT, 64, H * DH], F32, kind="ExternalOutput").ap()
    S = Sch(nc)
    with contextlib.ExitStack() as st:
        qt = [_sb(nc, st, [128, NSLOT * 64], BF16, "qt") for _ in range(2)]
        kt = [_sb(nc, st, [128, NKT], BF16, "kt") for _ in range(2)]
        va = [_sb(nc, st, [64, NPOOL, 129], BF16, "va") for _ in range(2)]
        vc = [_sb(nc, st, [128, 2, 129], BF16, "vc") for _ in range(2)]
        bs = [_sb(nc, st, [64, 3, 512], F32, "bs") for _ in range(2)]
        ost = [_sb(nc, st, [64, NSLOT, DH], F32, "ost") for _ in range(2)]
        NB, PD = 4, 2
        sb = [_sb(nc, st, [64, 512], F32, "sb") for _ in range(NB)]
        pl = [_sb(nc, st, [64, 512], BF16, "pl") for _ in range(NB)]
        pc = [_sb(nc, st, [128, 128], BF16, "pc") for _ in range(NB)]
        rc = [_sb(nc, st, [64, 1], F32, "rc") for _ in range(NB)]
        psl = [_ps(nc, st, [64, 512], F32, "psl") for _ in range(NB)]
        pcx = [_ps(nc, st, [128, 512], F32, "pcx") for _ in range(NB)]
        psc = [t[:, 0:128] for t in pcx]
        pso = [t[0:64, 256:385] for t in pcx]
        it = 0
        for h in range(H):
            hb = h % 2
            S.dma("pool", qt[hb][:], QT[h], writes=["qt%d" % hb])
            S.dma("pool", kt[hb][:], KT[h], writes=["kt%d" % hb])
            S.dma("pool", va[hb][:], VA[h], writes=["va%d" % hb])
            S.dma("pool", vc[hb][:], VC[h], writes=["vc%d" % hb])
            S.dma("sp", bs[hb][:], BS[h], writes=["bs%d" % hb])
            pend = []
            for s in list(range(NSLOT)) + [None] * PD:
                if s is not None:
                    b = it % NB
                    it += 1
                    if s < 14:
                        prow0, typ = s, 0
                    else:
                        prow0, typ = 21 + 8 * (s - 14), s - 13

                    def mm_s(e, b=b, hb=hb, s=s, prow0=prow0):
                        ins = None
                        for j in range(8):
                            ins = e.matmul(psl[b][0:64, j * 64:(j + 1) * 64],
                                           kt[hb][:, (prow0 + j) * 64:(prow0 + j + 1) * 64],
                                           qt[hb][:, s * 64:(s + 1) * 64], start=True, stop=True)
                        return ins
                    S.op("pe", mm_s, reads=["qt%d" % hb, "kt%d" % hb], writes=["psl%d" % b])

                    def mm_c(e, b=b, hb=hb, s=s):
                        ins = None
                        for cb in range(2):
                            ins = e.matmul(psc[b][:, cb * 64:(cb + 1) * 64],
                                           kt[hb][:, NPOOL * 64 + cb * 128:NPOOL * 64 + (cb + 1) * 128],
                                           qt[hb][:, s * 64:(s + 1) * 64], start=True, stop=True)
                        return ins
                    S.op("pe", mm_c, reads=["qt%d" % hb, "kt%d" % hb], writes=["psc%d" % b])
                    S.op("dve", lambda e, b=b, hb=hb, typ=typ: e.scalar_tensor_tensor(
                        out=sb[b][:], in0=psl[b][:], scalar=float(scale), in1=bs[hb][:, typ, :],
                        op0=ALU.mult, op1=ALU.add),
                        reads=["psl%d" % b, "bs%d" % hb], writes=["sb%d" % b])
                    S.op("act", lambda e, b=b: e.activation(out=pl[b][:], in_=sb[b][:], func=AF.Exp),
                         reads=["sb%d" % b], writes=["pl%d" % b])
                    S.op("act", lambda e, b=b: e.activation(out=pc[b][:], in_=psc[b], func=AF.Exp, scale=float(scale)),
                         reads=["psc%d" % b], writes=["pc%d" % b])

                    cur = (b, s, prow0)
                else:
                    cur = None
                pend.append(cur)
                if len(pend) > PD and pend[0] is not None:
                    b, s, prow0 = pend[0]
                    def mm_o(e, b=b, hb=hb, prow0=prow0):
                        ins = None
                        for j in range(8):
                            ins = e.matmul(pso[b], pl[b][0:64, j * 64:(j + 1) * 64],
                                           va[hb][0:64, prow0 + j, :], start=(j == 0), stop=False)
                        for cb in range(2):
                            ins = e.matmul(pso[b], pc[b][:, cb * 64:(cb + 1) * 64],
                                           vc[hb][:, cb, :], start=False, stop=(cb == 1))
                        return ins
                    S.op("pe", mm_o, reads=["pl%d" % b, "pc%d" % b, "va%d" % hb, "vc%d" % hb], writes=["pso%d" % b])
                    S.op("dve", lambda e, b=b: e.reciprocal(out=rc[b][:], in_=pso[b][:, 128:129]),
                         reads=["pso%d" % b], writes=["rc%d" % b])
                    S.op("dve", lambda e, b=b, hb=hb, s=s: e.tensor_scalar(
                        out=ost[hb][:, s, :], in0=pso[b][:, 0:128], scalar1=rc[b][:, 0:1], scalar2=None, op0=ALU.mult),
                        reads=["pso%d" % b, "rc%d" % b], writes=["ost%d" % hb])
                if len(pend) > PD:
                    pend.pop(0)
            S.dma("sp", O[:, :, h * DH:(h + 1) * DH].rearrange("s q d -> q s d"), ost[hb][:],
                  reads=["ost%d" % hb], is_output=True)
        S.emit()
    return nc


def dev_attn(maps):
    res = run_prog(("attn",), build_attn, maps)
    return [r["O"] for r in res]


def build_softmax(R, E):
    import contextlib
    nc = bass.Bass("TRN2", target_bir_lowering=False)
    T = R // 128
    X = nc.dram_tensor("X", [R, E], F32, kind="ExternalInput").ap()
    Y = nc.dram_tensor("Y", [R, E], F32, kind="ExternalOutput").ap()
    S = Sch(nc)
    with contextlib.ExitStack() as st:
        x = _sb(nc, st, [128, T, E], F32, "x")
        mx = _sb(nc, st, [128, T], F32, "mx")
        S.dma("sp", x[:], X.rearrange("(t p) e -> p t e", p=128), writes=["x"])
        S.op("dve", lambda e: e.tensor_reduce(out=mx[:], in_=x[:], axis=AX.X, op=ALU.max), reads=["x"], writes=["mx"])
        S.op("dve", lambda e: e.tensor_tensor(out=x[:], in0=x[:], in1=mx[:].unsqueeze(2).to_broadcast([128, T, E]),
                                              op=ALU.subtract), reads=["x", "mx"], writes=["x"])
        S.op("act", lambda e: e.activation(out=x[:], in_=x[:], func=AF.Exp), reads=["x"], writes=["x"])
        S.op("dve", lambda e: e.tensor_reduce(out=mx[:], in_=x[:], axis=AX.X, op=ALU.add), reads=["x"], writes=["mx"])
        S.op("dve", lambda e: e.reciprocal(out=mx[:], in_=mx[:]), reads=["mx"], writes=["mx"])
        S.op("dve", lambda e: e.tensor_tensor(out=x[:], in0=x[:], in1=mx[:].unsqueeze(2).to_broadcast([128, T, E]),
                                              op=ALU.mult), reads=["x", "mx"], writes=["x"])
        S.dma("sp", Y.rearrange("(t p) e -> p t e", p=128), x[:], reads=["x"], is_output=True)
        S.emit()
    return nc


def build_thresh(NR, NTOK, CAP, iters=36):
    import contextlib
    nc = bass.Bass("TRN2", target_bir_lowering=False)
    A = nc.dram_tensor("A", [NR, NTOK], F32, kind="ExternalInput").ap()
    M = nc.dram_tensor("M", [NR, NTOK], F32, kind="ExternalOutput").ap()
    S = Sch(nc)
    with contextlib.ExitStack() as st:
        a = _sb(nc, st, [NR, NTOK], F32, "a")
        c = _sb(nc, st, [NR, NTOK], F32, "c")
        lo = _sb(nc, st, [NR, 1], F32, "lo")
        hi = _sb(nc, st, [NR, 1], F32, "hi")
        mid = _sb(nc, st, [NR, 1], F32, "mid")
        cnt = _sb(nc, st, [NR, 1], F32, "cnt")
        d = _sb(nc, st, [NR, 1], F32, "d")
        S.dma("sp", a[:], A, writes=["a"])
        S.op("dve", lambda e: e.memset(lo[:], 0.0), writes=["lo"])
        S.op("dve", lambda e: e.memset(hi[:], 2.0), writes=["hi"])
        for _ in range(iters):
            S.op("dve", lambda e: e.tensor_tensor(out=mid[:], in0=lo[:], in1=hi[:], op=ALU.add),
                 reads=["lo", "hi"], writes=["mid"])
            S.op("dve", lambda e: e.tensor_scalar(out=mid[:], in0=mid[:], scalar1=0.5, scalar2=None, op0=ALU.mult),
                 reads=["mid"], writes=["mid"])
            S.op("dve", lambda e: e.tensor_scalar(out=c[:], in0=a[:], scalar1=mid[:, 0:1], scalar2=None, op0=ALU.is_ge),
                 reads=["a", "mid"], writes=["c"])
            S.op("dve", lambda e: e.tensor_reduce(out=cnt[:], in_=c[:], axis=AX.X, op=ALU.add),
                 reads=["c"], writes=["cnt"])
            S.op("dve", lambda e: e.tensor_scalar(out=cnt[:], in0=cnt[:], scalar1=float(CAP) - 0.5, scalar2=None,
                                                   op0=ALU.is_ge), reads=["cnt"], writes=["cnt"])
            S.op("dve", lambda e: e.tensor_tensor(out=d[:], in0=mid[:], in1=lo[:], op=ALU.subtract),
                 reads=["mid", "lo"], writes=["d"])
            S.op("dve", lambda e: e.scalar_tensor_tensor(out=lo[:], in0=d[:], scalar=cnt[:, 0:1], in1=lo[:],
                                                          op0=ALU.mult, op1=ALU.add),
                 reads=["d", "cnt", "lo"], writes=["lo"])
            S.op("dve", lambda e: e.tensor_tensor(out=d[:], in0=hi[:], in1=mid[:], op=ALU.subtract),
                 reads=["hi", "mid"], writes=["d"])
            S.op("dve", lambda e: e.scalar_tensor_tensor(out=hi[:], in0=d[:], scalar=cnt[:, 0:1], in1=mid[:],
                                                          op0=ALU.mult, op1=ALU.add),
                 reads=["d", "cnt", "mid"], writes=["hi"])
        S.op("dve", lambda e: e.tensor_scalar(out=c[:], in0=a[:], scalar1=lo[:, 0:1], scalar2=None, op0=ALU.is_ge),
             reads=["a", "lo"], writes=["c"])
        S.dma("sp", M, c[:], reads=["c"], is_output=True)
        S.emit()
    return nc


def dev_softmax(Xs):
    R, E = Xs[0].shape
    res = run_prog(("sm", R, E), lambda: build_softmax(R, E),
                   [{"X": np.ascontiguousarray(x, np.float32)} for x in Xs])
    return [r["Y"] for r in res]


def dev_thresh(As, cap):
    NR, NTOK = As[0].shape
    res = run_prog(("th", NR, NTOK, cap), lambda: build_thresh(NR, NTOK, cap),
                   [{"A": np.ascontiguousarray(a, np.float32)} for a in As])
    return [r["M"] for r in res]


D = 4096
NTOKB = 4096
GRID = 64
NH, DHD = 32, 128
NE, CAP, DFF = 16, 512, 1536
LMAX = 2560
LSEG = 512
POOLW = (2, 4, 8, 16)
ATT_INT0 = (4, 18, 32, 46)
ATT_SP = ((0, 1), (2, 3), (60, 61), (62, 63))
_EMU = [False]


def _T(a):
    return np.ascontiguousarray(np.swapaxes(a, -1, -2))


def _attn_rows(q):
    rows = [ATT_INT0[q] + s for s in range(14)] + list(ATT_SP[q])
    pool = [ATT_INT0[q] - 4 + w for w in range(21)]
    for r in ATT_SP[q]:
        r0 = min(max(r - 4, 0), GRID - 8)
        pool += [r0 + j for j in range(8)]
    return rows, pool


def _bias_tables(rpb, q):
    cols = np.arange(GRID)
    cstart = np.clip(cols - 8, 0, GRID - 16)
    valid = (cols[None, :] >= cstart[:, None]) & (cols[None, :] < cstart[:, None] + 16)
    dc = np.clip(cols[None, :] - cols[:, None] + 15, 0, 30)
    out = np.empty((NH, 64, 3, 512), np.float32)
    types = [None] + list(ATT_SP[q])
    for t, r in enumerate(types):
        if r is None:
            dr = np.arange(8) + 3
        else:
            r0 = min(max(r - 4, 0), GRID - 8)
            dr = r0 + np.arange(8) - r + 7
        B = rpb[:, dr[:, None, None], dc[None, :, :]]
        B = np.where(valid[None, None], B, np.float32(-30000.0))
        out[:, :, t, :] = B.transpose(0, 3, 1, 2).reshape(NH, 64, 512)
    return out


def _moe(hf, x_in, gvec, l, inp):
    c8 = range(NCORES)
    logits = dev_matmul([_T(hf[1024 * c:1024 * c + 1024])[None] for c in c8],
                        [inp["moe_w_router"][l][None]] * NCORES, fp32=True, NT=16)
    aff = np.concatenate(dev_softmax([y[0] for y in logits]), 0)
    AFT = aff.reshape(2, NTOKB, NE).transpose(0, 2, 1).reshape(2 * NE, NTOKB)
    mask = np.concatenate(dev_thresh([AFT[4 * c:4 * c + 4] for c in c8], CAP), 0)
    idx = np.zeros((2, NE, CAP), np.int64)
    for b in range(2):
        for e in range(NE):
            nz = np.nonzero(mask[b * NE + e] > 0.5)[0]
            idx[b, e, :min(CAP, len(nz))] = nz[:CAP]
    gidx = idx + (np.arange(2) * NTOKB)[:, None, None]
    ATs, RSs = [], []
    for c in c8:
        a = np.stack([np.concatenate([hf[gidx[0, e]], hf[gidx[1, e]]], 0) for e in (2 * c, 2 * c + 1)])
        ATs.append(_T(a))
        g = np.stack([np.concatenate([aff[gidx[0, e], e], aff[gidx[1, e], e]]) for e in (2 * c, 2 * c + 1)])
        RSs.append(g.reshape(2, 8, 128).transpose(0, 2, 1))
    hid = dev_matmul(ATs, [inp["moe_w_gate"][l][2 * c:2 * c + 2] for c in c8],
                     W2s=[inp["moe_w_up"][l][2 * c:2 * c + 2] for c in c8], NT=256)
    y = dev_matmul([_T(h) for h in hid], [inp["moe_w_down"][l][2 * c:2 * c + 2] for c in c8], RSs=RSs)
    yall = np.stack(y).reshape(NE, 2, CAP, D)
    ATc, Wc = [], []
    for c in c8:
        b, t0 = c // 4, (c % 4) * 1024
        yl = np.zeros((8, LSEG, D), np.float32)
        sel = np.zeros((8, LSEG, 128), np.float32)
        for tt in range(8):
            lo = t0 + 128 * tt
            ee, ss = np.nonzero((idx[b] >= lo) & (idx[b] < lo + 128))
            n = min(len(ee), LSEG)
            ee, ss = ee[:n], ss[:n]
            yl[tt, :n] = yall[ee, b, ss]
            sel[tt, np.arange(n), idx[b, ee, ss] - lo] = 1.0
        ATc.append(sel)
        Wc.append(yl)
    out = dev_matmul(ATc, Wc, fp32=True)
    return np.concatenate([o.reshape(1024, D) for o in out], 0)


def kernel(x, c, ctx, c_ctx, ada_w, ada_b, norm_mix_g, norm_ffn_g, na_w_qkv, na_q_gain, na_k_gain,
           na_rpb, na_w_out, pool_w, pool_scale, moe_w_router, moe_w_gate, moe_w_up, moe_w_down):
    inp = dict(moe_w_router=np.asarray(moe_w_router), moe_w_gate=np.asarray(moe_w_gate),
               moe_w_up=np.asarray(moe_w_up), moe_w_down=np.asarray(moe_w_down))
    f = lambda a: np.asarray(a, np.float32)
    x = f(x).reshape(2 * NTOKB, D)
    ctxf = f(ctx).reshape(512, D)
    c8 = range(NCORES)
    ones = np.ones(D, np.float32)
    zeros = np.zeros(D, np.float32)
    cond = np.stack([f(c)[0], f(c)[1], f(c_ctx)])
    ATa = np.stack([cond.T, cond.T])
    ada_w = np.asarray(ada_w)
    ada_b = f(ada_b)
    Ya = dev_matmul([ATa] * NCORES, [ada_w[:, :, 3072 * k:3072 * (k + 1)] for k in c8],
                    BIs=[ada_b[:, None, 3072 * k:3072 * (k + 1)] for k in c8], fp32=True, silu_a=True)
    mods = np.concatenate(Ya, -1).reshape(2, 3, 6, D)
    g_mix, g_ffn = f(norm_mix_g), f(norm_ffn_g)

    m0 = mods[0]
    Xs, VG, VSC, VSH = [], [], [], []
    for k in c8:
        b = k // 4
        Xs.append(np.concatenate([x[1024 * k:1024 * k + 1024], ctxf[64 * k:64 * k + 64], np.zeros((64, D), np.float32)]))
        VG.append(np.stack([g_mix[0], g_mix[0]]))
        VSC.append(np.stack([m0[b, 1], m0[2, 1]]))
        VSH.append(np.stack([m0[b, 0], m0[2, 0]]))
    Hh, _ = dev_normmod(Xs, VG, VSC, VSH, [0] * 8 + [1], D)
    Wq = np.asarray(na_w_qkv)[0][None]
    qkv = dev_matmul([_T(h)[None] for h in Hh], [Wq] * NCORES)
    qkv_lat = np.concatenate([y[0][:1024] for y in qkv], 0)
    qkv_ctx = np.concatenate([y[0][1024:1088] for y in qkv], 0)
    qg, kg = np.tile(f(na_q_gain)[0], NH), np.tile(f(na_k_gain)[0], NH)
    Xs = []
    for k in c8:
        sl = slice(1024 * k, 1024 * k + 1024)
        Xs.append(np.concatenate([qkv_lat[sl, 0:D], qkv_lat[sl, D:2 * D], qkv_ctx[64 * k:64 * k + 64, D:2 * D],
                                  np.zeros((64, D), np.float32)]))
    Hn, _ = dev_normmod(Xs, [np.stack([qg, kg])] * NCORES, [np.stack([zeros, zeros])] * NCORES,
                        [np.stack([zeros, zeros])] * NCORES, [0] * 8 + [1] * 9, DHD)
    qn = np.concatenate([h[0:1024] for h in Hn], 0)
    kn = np.concatenate([h[1024:2048] for h in Hn], 0)
    kcn = np.concatenate([h[2048:2112] for h in Hn], 0)
    vl = qkv_lat[:, 2 * D:3 * D]
    vcx = qkv_ctx[:, 2 * D:3 * D]
    rpb = f(na_rpb)[0]
    maps = []
    for k in c8:
        b, q = k // 4, k % 4
        rows, pool = _attn_rows(q)
        gq = qn[b * NTOKB:(b + 1) * NTOKB].reshape(GRID, GRID, NH, DHD)
        gk = kn[b * NTOKB:(b + 1) * NTOKB].reshape(GRID, GRID, NH, DHD)
        gv = vl[b * NTOKB:(b + 1) * NTOKB].reshape(GRID, GRID, NH, DHD)
        QT = gq[rows].transpose(2, 3, 0, 1).reshape(NH, DHD, NSLOT * 64)
        KTl = gk[pool].transpose(2, 3, 0, 1).reshape(NH, DHD, NPOOL * 64)
        KTc = kcn[256 * b:256 * b + 256].reshape(256, NH, DHD).transpose(1, 2, 0)
        VA = np.ones((NH, 64, NPOOL, 129), np.float32)
        VA[..., :128] = gv[pool].transpose(2, 1, 0, 3)
        VC = np.ones((NH, 128, 2, 129), np.float32)
        VC[..., :128] = vcx[256 * b:256 * b + 256].reshape(2, 128, NH, DHD).transpose(2, 1, 0, 3)
        maps.append({"QT": np.ascontiguousarray(QT), "KT": np.ascontiguousarray(np.concatenate([KTl, KTc], -1)),
                     "VA": VA, "VC": VC, "BS": _bias_tables(rpb, q)})
    Oc = dev_attn(maps)
    o_all = np.zeros((2, GRID, GRID, D), np.float32)
    for k in c8:
        rows, _ = _attn_rows(k % 4)
        o_all[k // 4, rows] = Oc[k]
    o_all = o_all.reshape(2 * NTOKB, D)
    Wo = np.asarray(na_w_out)[0][None]
    ya = dev_matmul([_T(o_all[1024 * k:1024 * k + 1024])[None] for k in c8], [Wo] * NCORES)
    hf, x1 = dev_normmod([x[1024 * k:1024 * k + 1024] for k in c8],
                         [g_ffn[0][None]] * NCORES, [m0[k // 4, 4][None] for k in c8], [m0[k // 4, 3][None] for k in c8],
                         [0] * 8, D, YRs=[y[0] for y in ya], V1=[m0[k // 4, 2][None] for k in c8], V2=[ones[None]] * NCORES)
    hf, x1 = np.concatenate(hf, 0), np.concatenate(x1, 0)
    mo = _moe(hf, x1, None, 0, inp)

    m1 = mods[1]
    h1, x2 = dev_normmod([x1[1024 * k:1024 * k + 1024] for k in c8],
                         [g_mix[1][None]] * NCORES, [m1[k // 4, 1][None] for k in c8], [m1[k // 4, 0][None] for k in c8],
                         [0] * 8, D, YRs=[mo[1024 * k:1024 * k + 1024] for k in c8],
                         V1=[m0[k // 4, 5][None] for k in c8], V2=[ones[None]] * NCORES)
    h1, x2 = np.concatenate(h1, 0), np.concatenate(x2, 0)
    ATp, Wp = [], []
    for k in c8:
        b, n0 = k // 4, (k % 4) * 1024
        pos = np.arange(n0 - 64, n0 + 1088)
        inb = (pos >= 0) & (pos < NTOKB)
        hh = np.zeros((1152, D), np.float32)
        hh[inb] = h1[b * NTOKB + pos[inb]]
        A = np.zeros((4, 1152, 1024), np.float32)
        n = n0 + np.arange(1024)
        for g, w in enumerate(POOLW):
            lo = np.clip(n - w // 2, 0, NTOKB)
            hi = np.clip(n + w // 2, 0, NTOKB)
            inv = (1.0 / (hi - lo)).astype(np.float32)
            for t in range(1024):
                A[g, lo[t] - (n0 - 64):hi[t] - (n0 - 64), t] = inv[t]
                A[g, n[t] - (n0 - 64), t] -= 1.0
        ATp.append(A)
        Wp.append(np.ascontiguousarray(hh.reshape(1152, 4, 1024).transpose(1, 0, 2)))
    pooled = dev_matmul(ATp, Wp, fp32=True, band=(0, 1))
    yp = dev_matmul([_T(p) for p in pooled], [np.asarray(pool_w)[0]] * NCORES)
    yp = [np.ascontiguousarray(y.transpose(1, 0, 2).reshape(1024, D)) for y in yp]
    ps = f(pool_scale)[0]
    hf, x3 = dev_normmod([x2[1024 * k:1024 * k + 1024] for k in c8],
                         [g_ffn[1][None]] * NCORES, [m1[k // 4, 4][None] for k in c8], [m1[k // 4, 3][None] for k in c8],
                         [0] * 8, D, YRs=yp, V1=[m1[k // 4, 2][None] for k in c8], V2=[ps[None]] * NCORES)
    hf, x3 = np.concatenate(hf, 0), np.concatenate(x3, 0)
    mo = _moe(hf, x3, None, 1, inp)
    _, x4 = dev_normmod([x3[1024 * k:1024 * k + 1024] for k in c8],
                        [ones[None]] * NCORES, [zeros[None]] * NCORES, [zeros[None]] * NCORES,
                        [0] * 8, D, YRs=[mo[1024 * k:1024 * k + 1024] for k in c8],
                        V1=[m1[k // 4, 5][None] for k in c8], V2=[ones[None]] * NCORES)
    return np.concatenate(x4, 0).reshape(2, NTOKB, D).astype(np.float32)
```
